# Optimizing a Trainium2 kernel written in Bass

```python
import math
import jax
import jax.numpy as jnp
from jax import lax
import numpy as np

D_MODEL = 2048
BATCH = 1
SEQ = 16384
DEPTH = 1

MIX_WIDTH = D_MODEL
ATTN_WIDTH = MIX_WIDTH // 2
SSM_WIDTH = MIX_WIDTH - ATTN_WIDTH
HEAD_DIM = 64
N_HEADS = ATTN_WIDTH // HEAD_DIM
ATTN_BRANCHES = ((128, 1), (512, 4), (2048, 16))
SSM_GROUP_CH = 16
N_SSM_GROUPS = SSM_WIDTH // SSM_GROUP_CH
SSM_STATE = 64
DT_MIN = 0.001
DT_MAX = 0.1
IN_WIDTH = 3 * ATTN_WIDTH + SSM_WIDTH
N_EXPERTS = 32
TOP_K = 4
D_FF_EXPERT = D_MODEL
SWIGLU_LIMIT = 7.0
SWIGLU_ALPHA = 1.702
EXPERT_BLOCK = 128
NORM_EPS = 1e-6

kernel_name = 'hymba_s5_longnet_moe_block'


def rmsnorm(x, g):
    xf = x.astype(jnp.float32)
    y = xf * lax.rsqrt(jnp.mean(xf * xf, axis=-1, keepdims=True) + NORM_EPS)
    return (y * g.astype(jnp.float32)).astype(x.dtype)


def dilated_branch(q, k, v, window, dil):
    Bb, S, H, Dh = q.shape
    blk = window // dil
    span = blk * dil
    nb = -(-S // span)
    Sp = nb * span
    padw = ((0, 0), (0, Sp - S), (0, 0), (0, 0))

    def split_stride(t):
        return jnp.pad(t, padw).reshape(Bb, nb, blk, dil, H, Dh)

    def with_prev(t):
        prev = jnp.pad(t[:, :-1], ((0, 0), (1, 0), (0, 0), (0, 0), (0, 0), (0, 0)))
        return jnp.concatenate([prev, t], axis=2)

    qb = split_stride(q)
    kc = with_prev(split_stride(k))
    vc = with_prev(split_stride(v))
    s = jnp.einsum('bnqrhd,bnkrhd->bnrhqk', qb, kc,
                   preferred_element_type=jnp.float32) * (Dh ** -0.5)
    qi = jnp.arange(blk)[:, None]
    kj = jnp.arange(2 * blk)[None, :]
    dist = qi + blk - kj
    band = (dist >= 0) & (dist <= blk)
    first = (jnp.arange(nb)[:, None, None] > 0) | (kj[None] >= blk)
    mask = (band[None] & first)[None, :, None, None]
    s = jnp.where(mask, s, -jnp.inf)
    m = jnp.max(s, axis=-1, keepdims=True)
    p = jnp.exp(s - m)
    den = jnp.sum(p, axis=-1, keepdims=True)
    o = jnp.einsum('bnrhqk,bnkrhd->bnrhqd', p, vc.astype(jnp.float32)) / den
    lse = (m + jnp.log(den))[..., 0]
    o = jnp.transpose(o, (0, 1, 4, 2, 3, 5)).reshape(Bb, Sp, H, Dh)[:, :S]
    lse = jnp.transpose(lse, (0, 1, 4, 2, 3)).reshape(Bb, Sp, H)[:, :S]
    return o, lse


def dilated_mixture_attention(q, k, v):
    outs = []
    lses = []
    for window, dil in ATTN_BRANCHES:
        o, lse = dilated_branch(q, k, v, window, dil)
        outs.append(o)
        lses.append(lse)
    wts = jax.nn.softmax(jnp.stack(lses, axis=0), axis=0)[..., None]
    o = jnp.sum(wts * jnp.stack(outs, axis=0), axis=0)
    return o.astype(q.dtype)


def s5_mixer(u, lam_re, lam_im, b_re, b_im, c_re, c_im, d_skip, log_dt, w_glu, b_glu):
    Bb, S, W = u.shape
    f32 = jnp.float32
    uf = u.astype(f32).reshape(Bb, S, N_SSM_GROUPS, SSM_GROUP_CH)
    dt = jnp.exp(log_dt.astype(f32))[:, None]
    lr = lam_re.astype(f32)
    li = lam_im.astype(f32)
    mag = jnp.exp(lr * dt)
    ab_re = mag * jnp.cos(li * dt)
    ab_im = mag * jnp.sin(li * dt)
    den = lr * lr + li * li
    nr = ab_re - 1.0
    ni = ab_im
    f_re = (nr * lr + ni * li) / den
    f_im = (ni * lr - nr * li) / den
    br = b_re.astype(f32)
    bi = b_im.astype(f32)
    bb_re = f_re[..., None] * br - f_im[..., None] * bi
    bb_im = f_re[..., None] * bi + f_im[..., None] * br
    bu_re = jnp.einsum('gnp,bsgp->bsgn', bb_re, uf)
    bu_im = jnp.einsum('gnp,bsgp->bsgn', bb_im, uf)
    a_re = jnp.broadcast_to(ab_re, bu_re.shape)
    a_im = jnp.broadcast_to(ab_im, bu_im.shape)

    def combine(e1, e2):
        a1r, a1i, b1r, b1i = e1
        a2r, a2i, b2r, b2i = e2
        return (a2r * a1r - a2i * a1i,
                a2r * a1i + a2i * a1r,
                a2r * b1r - a2i * b1i + b2r,
                a2r * b1i + a2i * b1r + b2i)

    _, _, xr, xi = lax.associative_scan(combine, (a_re, a_im, bu_re, bu_im), axis=1)
    y = (jnp.einsum('gpn,bsgn->bsgp', c_re.astype(f32), xr)
         - jnp.einsum('gpn,bsgn->bsgp', c_im.astype(f32), xi)
         + d_skip.astype(f32) * uf)
    y = jax.nn.gelu(y.reshape(Bb, S, W))
    y = y * jax.nn.sigmoid(y @ w_glu.astype(f32) + b_glu.astype(f32))
    return y.astype(u.dtype)


def clamped_swiglu(gu):
    x_glu = jnp.minimum(gu[..., ::2], SWIGLU_LIMIT)
    x_lin = jnp.clip(gu[..., 1::2], -SWIGLU_LIMIT, SWIGLU_LIMIT)
    return x_glu * jax.nn.sigmoid(SWIGLU_ALPHA * x_glu) * (x_lin + 1.0)


def moe_ffn(h, l, w_router, b_router, w_gate_up, b_gate_up, w_down, b_down):
    Bb, S, D = h.shape
    T = Bb * S
    hf = h.reshape(T, D)
    logits = (hf @ w_router[l] + b_router[l]).astype(jnp.float32)
    top_vals, top_idx = lax.top_k(logits, TOP_K)
    gates = jax.nn.softmax(top_vals, axis=-1)
    n_assign = T * TOP_K
    flat_e = top_idx.reshape(-1).astype(jnp.int32)
    flat_tok = jnp.arange(n_assign, dtype=jnp.int32) // TOP_K
    flat_gate = gates.reshape(-1)
    order = jnp.argsort(flat_e)
    sorted_e = flat_e[order]
    sorted_tok = flat_tok[order]
    sorted_gate = flat_gate[order]
    counts = jnp.bincount(flat_e, length=N_EXPERTS).astype(jnp.int32)
    start = jnp.cumsum(counts) - counts
    padded = (counts + EXPERT_BLOCK - 1) // EXPERT_BLOCK * EXPERT_BLOCK
    pad_end = jnp.cumsum(padded)
    pad_start = pad_end - padded
    rank = jnp.arange(n_assign, dtype=jnp.int32) - start[sorted_e]
    dest = pad_start[sorted_e] + rank
    m_pad = n_assign + N_EXPERTS * EXPERT_BLOCK
    n_blocks = m_pad // EXPERT_BLOCK
    row_tok = jnp.full((m_pad,), T, jnp.int32).at[dest].set(sorted_tok)
    row_gate = jnp.zeros((m_pad,), jnp.float32).at[dest].set(sorted_gate)
    block_e = jnp.minimum(
        jnp.searchsorted(pad_end, jnp.arange(n_blocks, dtype=jnp.int32) * EXPERT_BLOCK,
                         side='right'), N_EXPERTS - 1).astype(jnp.int32)
    h_pad = jnp.concatenate([hf, jnp.zeros((1, D), hf.dtype)], axis=0)

    def expert_block(args):
        e, tok, g = args
        xb = h_pad[tok]
        gu = xb @ w_gate_up[l, e] + b_gate_up[l, e]
        yb = clamped_swiglu(gu) @ w_down[l, e] + b_down[l, e]
        return yb * g[:, None].astype(yb.dtype)

    out = lax.map(expert_block, (block_e,
                                 row_tok.reshape(n_blocks, EXPERT_BLOCK),
                                 row_gate.reshape(n_blocks, EXPERT_BLOCK)))
    y = jax.ops.segment_sum(out.reshape(m_pad, D), row_tok, num_segments=T + 1)[:T]
    return y.reshape(Bb, S, D).astype(h.dtype)


def setup_inputs(seed: int = 0) -> dict:
    key = jax.random.key(seed)
    ks = jax.random.split(key, 30)
    f32 = jnp.float32
    L, D, A, W = DEPTH, D_MODEL, ATTN_WIDTH, SSM_WIDTH
    G, N, P = N_SSM_GROUPS, SSM_STATE, SSM_GROUP_CH
    E, F = N_EXPERTS, D_FF_EXPERT

    def nrm(k, shape, scale):
        return jax.random.normal(k, shape, f32) * scale

    n_idx = jnp.arange(N, dtype=f32)[None, None, :]
    return {
        'x': nrm(ks[0], (BATCH, SEQ, D), 1.0),
        'c': nrm(ks[1], (BATCH, D), 1.0),
        'w_ada': nrm(ks[2], (L, D, 6 * D), D ** -0.5),
        'b_ada': nrm(ks[3], (L, 6 * D), 0.02),
        'norm1_g': 1.0 + nrm(ks[4], (L, D), 0.02),
        'w_in': nrm(ks[5], (L, D, IN_WIDTH), D ** -0.5),
        'lambda_re': -0.5 + nrm(ks[6], (L, G, N), 0.01),
        'lambda_im': jnp.pi * n_idx + nrm(ks[7], (L, G, N), 0.01),
        'ssm_b_re': nrm(ks[8], (L, G, N, P), (2 * P) ** -0.5),
        'ssm_b_im': nrm(ks[9], (L, G, N, P), (2 * P) ** -0.5),
        'ssm_c_re': nrm(ks[10], (L, G, P, N), (2 * N) ** -0.5),
        'ssm_c_im': nrm(ks[11], (L, G, P, N), (2 * N) ** -0.5),
        'ssm_d': nrm(ks[12], (L, G, P), 1.0),
        'ssm_log_dt': jax.random.uniform(ks[13], (L, G), f32, math.log(DT_MIN), math.log(DT_MAX)),
        'w_glu': nrm(ks[14], (L, W, W), W ** -0.5),
        'b_glu': nrm(ks[15], (L, W), 0.02),
        'attn_out_g': 1.0 + nrm(ks[16], (L, A), 0.02),
        'ssm_out_g': 1.0 + nrm(ks[17], (L, W), 0.02),
        'w_out': nrm(ks[18], (L, MIX_WIDTH, D), MIX_WIDTH ** -0.5),
        'norm2_g': 1.0 + nrm(ks[19], (L, D), 0.02),
        'w_router': nrm(ks[20], (L, D, E), D ** -0.5),
        'b_router': nrm(ks[21], (L, E), 0.01),
        'w_gate_up': nrm(ks[22], (L, E, D, 2 * F), D ** -0.5),
        'b_gate_up': nrm(ks[23], (L, E, 2 * F), 0.02),
        'w_down': nrm(ks[24], (L, E, F, D), F ** -0.5),
        'b_down': nrm(ks[25], (L, E, D), 0.02),
        'final_g': 1.0 + nrm(ks[26], (D,), 0.02),
    }


def reference(x, c, w_ada, b_ada, norm1_g, w_in, lambda_re, lambda_im, ssm_b_re, ssm_b_im,
              ssm_c_re, ssm_c_im, ssm_d, ssm_log_dt, w_glu, b_glu, attn_out_g, ssm_out_g,
              w_out, norm2_g, w_router, b_router, w_gate_up, b_gate_up, w_down, b_down,
              final_g):
    Bb, S, _ = x.shape
    c_act = jax.nn.silu(c)
    for l in range(DEPTH):
        mod = c_act @ w_ada[l] + b_ada[l]
        sh1, sc1, g1, sh2, sc2, g2 = [m[:, None, :] for m in jnp.split(mod, 6, axis=-1)]
        h = rmsnorm(x, norm1_g[l]) * (1.0 + sc1) + sh1
        proj = h @ w_in[l]
        q, k, v, u = jnp.split(proj, [ATTN_WIDTH, 2 * ATTN_WIDTH, 3 * ATTN_WIDTH], axis=-1)
        q = q.reshape(Bb, S, N_HEADS, HEAD_DIM)
        k = k.reshape(Bb, S, N_HEADS, HEAD_DIM)
        v = v.reshape(Bb, S, N_HEADS, HEAD_DIM)
        y_attn = dilated_mixture_attention(q, k, v).reshape(Bb, S, ATTN_WIDTH)
        y_ssm = s5_mixer(u, lambda_re[l], lambda_im[l], ssm_b_re[l], ssm_b_im[l],
                         ssm_c_re[l], ssm_c_im[l], ssm_d[l], ssm_log_dt[l], w_glu[l], b_glu[l])
        y_mix = jnp.concatenate([rmsnorm(y_attn, attn_out_g[l]),
                                 rmsnorm(y_ssm, ssm_out_g[l])], axis=-1)
        x = x + g1 * (y_mix @ w_out[l])
        h2 = rmsnorm(x, norm2_g[l]) * (1.0 + sc2) + sh2
        x = x + g2 * moe_ffn(h2, l, w_router, b_router, w_gate_up, b_gate_up, w_down, b_down)
    return rmsnorm(x, final_g)
```

```python
import contextlib
import math
import numpy as np
import concourse.bass as bass
import concourse.mybir as mybir
from concourse.bass_utils import run_bass_kernel_spmd

F32 = mybir.dt.float32
BF16 = mybir.dt.bfloat16
I32 = mybir.dt.int32
ALU = mybir.AluOpType
AF = mybir.ActivationFunctionType
AX = mybir.AxisListType

NCORES = 8
HEAD_DIM = 64
BRANCH_DIL = (1, 4, 16)
NORM_EPS = 1e-6
SW_LIMIT = 7.0
SW_ALPHA = 1.702
TOP_K = 4


class Cfg:
    def __init__(self, D=2048, TOK=2048, E=32, F=None, debug=False):
        self.debug = debug
        self.D = D
        self.TOK = TOK
        self.NCH = NCORES
        self.A = D // 2
        self.W = D // 2
        self.NHP = self.A // 128
        self.WC = self.W // 128
        self.NT = self.W // 32
        self.DC = D // 128
        self.E = E
        self.F = F or D
        self.FC = self.F // 128
        self.INC = 3 * self.NHP + self.WC


class Buf:
    __slots__ = ("t", "last_w", "readers")

    def __init__(self, t):
        self.t = t
        self.last_w = None
        self.readers = []

    def __getitem__(self, k):
        return self.t[k]


class Prog:
    NS = 8

    def __init__(self, nc):
        self.nc = nc
        self.es = contextlib.ExitStack()
        self.stacks = [self.es]
        self.eng = dict(pe=nc.tensor, act=nc.scalar, dve=nc.vector, pool=nc.gpsimd, sp=nc.sync)
        self.csem = {e: self.es.enter_context(nc.semaphore("c_" + e)) for e in ("pe", "act", "dve", "pool")}
        self.ccnt = {e: 0 for e in self.csem}
        self.dsem = {q: [self.es.enter_context(nc.semaphore("d_%s%d" % (q, i))) for i in range(self.NS)]
                     for q in ("sp", "pool", "act")}
        self.dcnt = {q: 0 for q in self.dsem}
        self.seen = {e: {} for e in self.eng}
        self.nbuf = 0
        self.psF = []
        self.psi = 0

    def push(self):
        s = contextlib.ExitStack()
        self.stacks.append(s)

    def pop(self):
        self.barrier()
        self.stacks.pop().close()

    def sb(self, shape, dt, name="sb"):
        self.nbuf += 1
        t = self.stacks[-1].enter_context(self.nc.sbuf_tensor("%s_%d" % (name, self.nbuf), list(shape), dt))
        return Buf(t)

    def ps(self, shape, dt, name="ps"):
        self.nbuf += 1
        t = self.stacks[-1].enter_context(self.nc.psum_tensor("%s_%d" % (name, self.nbuf), list(shape), dt))
        return Buf(t)

    def dram(self, name, shape, dt, kind="Internal"):
        t = self.nc.dram_tensor(name, list(shape), dt, kind=kind)
        return Buf(t.ap())

    def next_ps(self):
        b = self.psF[self.psi % len(self.psF)]
        self.psi += 1
        return b

    def _wait(self, e, ev):
        sem, val, _ = ev
        k = id(sem)
        if self.seen[e].get(k, 0) >= val:
            return
        self.eng[e].wait_ge(sem, val)
        self.seen[e][k] = val

    def _deps(self, e, reads, writes):
        evs = []
        for b in reads:
            if b.last_w is not None:
                evs.append(b.last_w)
        for b in writes:
            if b.last_w is not None:
                evs.append(b.last_w)
            evs.extend(b.readers)
        for ev in evs:
            if e == "pe" and ev[2] == "pe":
                continue
            self._wait(e, ev)

    def _commit(self, ev, reads, writes):
        for b in writes:
            b.last_w = ev
            b.readers = []
        for b in reads:
            if b not in writes:
                b.readers.append(ev)
                if len(b.readers) > 96:
                    best = {}
                    for r in b.readers:
                        k = id(r[0])
                        if k not in best or best[k][1] < r[1]:
                            best[k] = r
                    b.readers = list(best.values())

    def op(self, e, fn, reads=(), writes=()):
        self._deps(e, reads, writes)
        ins = fn(self.eng[e])
        self.ccnt[e] += 1
        ins.then_inc(self.csem[e], 1)
        ev = (self.csem[e], self.ccnt[e], e)
        self._commit(ev, reads, writes)
        return ev

    def dma(self, q, out, in_, reads=(), writes=(), **kw):
        self._deps(q, reads, writes)
        i = self.dcnt[q]
        slot = i % self.NS
        rnd = i // self.NS
        sem = self.dsem[q][slot]
        if rnd > 0:
            self._wait(q, (sem, 16 * rnd, "dma"))
        ins = self.eng[q].dma_start(out=out, in_=in_, **kw)
        ins.then_inc(sem, 16)
        self.dcnt[q] += 1
        ev = (sem, 16 * (rnd + 1), "dma")
        self._commit(ev, reads, writes)
        return ev

    def barrier(self):
        evs = []
        for e in self.csem:
            if self.ccnt[e] > 0:
                evs.append((self.csem[e], self.ccnt[e], e))
        for q in self.dsem:
            for s in range(self.NS):
                n = (self.dcnt[q] - s + self.NS - 1) // self.NS
                if n > 0:
                    evs.append((self.dsem[q][s], 16 * n, "dma"))
        for e in self.eng:
            for ev in evs:
                self._wait(e, ev)

    def close(self):
        self.barrier()
        self.es.close()


def build_program(cfg):
    D, TOK, NCH, A, W = cfg.D, cfg.TOK, cfg.NCH, cfg.A, cfg.W
    NHP, WC, NT, DC, E, F, FC, INC = cfg.NHP, cfg.WC, cfg.NT, cfg.DC, cfg.E, cfg.F, cfg.FC, cfg.INC
    NTT = TOK // 128
    NST = TOK // 512
    TALL = NCH * TOK

    nc = bass.Bass("TRN2", target_bir_lowering=False)
    P = Prog(nc)

    def din(name, shape, dt=F32):
        return P.dram(name, shape, dt, kind="ExternalInput")

    x_ext = din("x_ext", [TALL, D])
    valid_in = din("valid", [128, NCH])
    mfirst_in = din("mask_first", [128, 256])
    mfull_in = din("mask_full", [128, 256])
    ident_in = din("ident", [128, 128])
    c_col_in = din("c_col", [128, DC])
    w_ada_l = din("w_ada_l", [6 * DC, 128, DC * 128])
    b_ada_col = din("b_ada_col", [128, 6 * DC])
    n1g_col = din("n1g_col", [128, DC])
    n2g_col = din("n2g_col", [128, DC])
    fg_row = din("fg_row", [1, D])
    w_in_l = din("w_in_l", [INC, 128, DC * 128])
    lam_re_l = din("lam_re_l", [128, NT])
    lam_im_l = din("lam_im_l", [128, NT])
    logdt_l = din("logdt_l", [128, NT])
    bre_l = din("bre_l", [128, NT * 128])
    bim_l = din("bim_l", [128, NT * 128])
    cre_l = din("cre_l", [128, NT * 128])
    cim_l = din("cim_l", [128, NT * 128])
    dskip_col = din("dskip_col", [128, WC])
    w_glu_l = din("w_glu_l", [128, WC * W])
    b_glu_col = din("b_glu_col", [128, WC])
    ag_col = din("ag_col", [128, NHP])
    sg_col = din("sg_col", [128, WC])
    w_out_l = din("w_out_l", [128, (NHP + WC) * D])
    w_r_l = din("w_r_l", [128, DC * E])
    b_r_row = din("b_r_row", [1, E])
    w_gate_l = din("w_gate_l", [E * FC, 128, DC * 128])
    w_lin_l = din("w_lin_l", [E * FC, 128, DC * 128])
    bg_col = din("bg_col", [128, E * FC])
    bl_col = din("bl_col", [128, E * FC])
    w_down_l = din("w_down_l", [E * FC, 128, D])
    b_down = din("b_down", [E, D])
    out_d = P.dram("out", [TOK, D], F32, kind="ExternalOutput")

    SK = "ExternalOutput" if cfg.debug else "Internal"
    PT = P.dram("PT", [INC * 128, TALL], BF16, kind=SK)
    PTk = [Buf(None) for _ in range(INC)]
    X2k = [Buf(None) for _ in range(NTT)]
    outk = [Buf(None) for _ in range(NTT)]
    modrow = P.dram("modrow", [6 * DC, 128], F32, kind=SK)
    X2 = P.dram("X2", [TOK, D], F32, kind=SK)
    H2T = P.dram("H2T", [DC * 128, TOK], BF16, kind=SK)
    DBG = P.dram("DBG", [128, 8 * TOK], F32, kind=SK)

    P.psF = [P.ps([128, 512], F32, "psF") for _ in range(6)]
    psB = [P.ps([128, 1024], BF16, "psB") for _ in range(2)]

    ident_f = P.sb([128, 128], F32, "identf")
    ident_b = P.sb([128, 128], BF16, "identb")
    ones_f = P.sb([128, 128], F32, "onesf")
    ones_b = P.sb([128, 128], BF16, "onesb")
    valid = P.sb([128, NCH], F32, "valid")
    modT = P.sb([128, 6 * DC], F32, "modT")
    s1c = P.sb([128, DC], F32, "s1c")
    s2c = P.sb([128, DC], F32, "s2c")
    G = P.sb([128, NTT * E], F32, "gates")
    rstd_a = P.sb([128, NTT], F32, "rstda")
    rstd_s = P.sb([128, NTT], F32, "rstds")

    P.dma("sp", ident_f[:], ident_in[:, :], reads=[ident_in], writes=[ident_f])
    P.dma("sp", valid[:], valid_in[:, :], reads=[valid_in], writes=[valid])
    P.op("dve", lambda e: e.tensor_copy(out=ident_b[:], in_=ident_f[:]), reads=[ident_f], writes=[ident_b])
    P.op("dve", lambda e: e.memset(ones_f[:], 1.0), writes=[ones_f])
    P.op("dve", lambda e: e.memset(ones_b[:], 1.0), writes=[ones_b])

    def rstd_from_ss(ssb, ss, n, outb, out, width=1):
        t = P.sb([128, width], F32, "rs_t")
        P.op("dve", lambda e: e.tensor_scalar(out=t[:], in0=ss, scalar1=1.0 / n, scalar2=NORM_EPS,
                                               op0=ALU.mult, op1=ALU.add), reads=[ssb], writes=[t])
        P.op("act", lambda e: e.activation(out=t[:], in_=t[:], func=AF.Sqrt), reads=[t], writes=[t])
        P.op("dve", lambda e: e.reciprocal(out=out, in_=t[:]), reads=[t], writes=[outb])

    P.push()
    c_act = P.sb([128, DC], F32, "cact")
    b_ada = P.sb([128, 6 * DC], F32, "bada")
    n1g = P.sb([128, DC], F32, "n1g")
    n2g = P.sb([128, DC], F32, "n2g")
    P.dma("sp", c_act[:], c_col_in[:, :], reads=[c_col_in], writes=[c_act])
    P.dma("sp", b_ada[:], b_ada_col[:, :], reads=[b_ada_col], writes=[b_ada])
    P.dma("sp", n1g[:], n1g_col[:, :], reads=[n1g_col], writes=[n1g])
    P.dma("sp", n2g[:], n2g_col[:, :], reads=[n2g_col], writes=[n2g])
    P.op("act", lambda e: e.activation(out=c_act[:], in_=c_act[:], func=AF.Silu), reads=[c_act], writes=[c_act])
    wa = [P.sb([128, DC * 128], F32, "wa") for _ in range(2)]
    mps = P.next_ps()
    for oc in range(6 * DC):
        wt = wa[oc % 2]
        P.dma("sp", wt[:], w_ada_l[oc, :, :], reads=[w_ada_l], writes=[wt])
        for k in range(DC):
            P.op("pe", lambda e: e.matmul(mps[:, oc:oc + 1], lhsT=wt[:, k * 128:(k + 1) * 128],
                                          rhs=c_act[:, k:k + 1], start=(k == 0), stop=(k == DC - 1)),
                 reads=[wt, c_act], writes=[mps])
    P.op("dve", lambda e: e.tensor_tensor(out=modT[:], in0=mps[:, 0:6 * DC], in1=b_ada[:], op=ALU.add),
         reads=[mps, b_ada], writes=[modT])
    P.op("dve", lambda e: e.scalar_tensor_tensor(out=s1c[:], in0=modT[:, DC:2 * DC], scalar=1.0, in1=n1g[:],
                                                 op0=ALU.add, op1=ALU.mult), reads=[modT, n1g], writes=[s1c])
    P.op("dve", lambda e: e.scalar_tensor_tensor(out=s2c[:], in0=modT[:, 4 * DC:5 * DC], scalar=1.0, in1=n2g[:],
                                                 op0=ALU.add, op1=ALU.mult), reads=[modT, n2g], writes=[s2c])
    tps = P.next_ps()
    P.op("pe", lambda e: e.transpose(tps[0:6 * DC, 0:128], modT[:, :], ident_f[:]), reads=[modT, ident_f], writes=[tps])
    mrow = P.sb([128, 128], F32, "mrow")
    P.op("dve", lambda e: e.tensor_copy(out=mrow[0:6 * DC, :], in_=tps[0:6 * DC, 0:128]), reads=[tps], writes=[mrow])
    P.dma("sp", modrow[:, :], mrow[0:6 * DC, :], reads=[mrow], writes=[modrow])
    P.pop()

    P.push()
    wu = P.sb([128, WC * DC * 128], BF16, "wu")
    for j in range(WC):
        P.dma("pool", wu[:, j * DC * 128:(j + 1) * DC * 128], w_in_l[3 * NHP + j, :, :], reads=[w_in_l], writes=[wu])
    xt = [P.sb([128, D], F32, "xt") for _ in range(4)]
    xn = [P.sb([128, D], BF16, "xn") for _ in range(4)]
    hT = [P.sb([128, DC * 512], BF16, "hT") for _ in range(2)]
    wq = [P.sb([128, DC * 128], BF16, "wq") for _ in range(3)]
    stg = [P.sb([128, 512], BF16, "stg") for _ in range(4)]
    ssqs = [P.sb([128, 1], F32, "ssq") for _ in range(8)]
    rsds = [P.sb([128, 1], F32, "rsd") for _ in range(8)]
    junks = [P.sb([128, D], BF16, "junk") for _ in range(2)]
    cnt = dict(tile=0, w=0, st=0, stg=0, pb=0)
    for jc in range(NCH):
        if jc < NCH - 2:
            ccs = list(range(3 * NHP, INC))
        elif jc == NCH - 2:
            ccs = list(range(NHP, INC))
        else:
            ccs = list(range(INC))
        for st in range(NST):
            h = hT[cnt["st"] % 2]
            cnt["st"] += 1
            for t4 in range(4):
                i = t4
                cnt["tile"] += 1
                row0 = jc * TOK + st * 512 + t4 * 128
                P.dma("sp", xt[i][:], x_ext[row0:row0 + 128, :], reads=[x_ext], writes=[xt[i]])
                ssq = ssqs[cnt["tile"] % 8]
                rsd = rsds[cnt["tile"] % 8]
                junk = junks[t4 % 2]
                P.op("act", lambda e: e.activation(out=junk[:], in_=xt[i][:], func=AF.Square,
                                                   accum_out=ssq[:, 0:1]), reads=[xt[i]], writes=[junk, ssq])
                rstd_from_ss(ssq, ssq[:, 0:1], D, rsd, rsd[:, 0:1])
                P.op("dve", lambda e: e.tensor_scalar(out=xn[i][:], in0=xt[i][:], scalar1=rsd[:, 0:1],
                                                      scalar2=None, op0=ALU.mult), reads=[xt[i], rsd], writes=[xn[i]])
            for t4 in range(4):
                i = t4
                for kb in range(0, DC, 8):
                    nk = min(8, DC - kb)
                    pb = psB[cnt["pb"] % 2]
                    cnt["pb"] += 1
                    for kk in range(nk):
                        k = kb + kk
                        P.op("pe", lambda e: e.transpose(pb[:, kk * 128:(kk + 1) * 128], xn[i][:, k * 128:(k + 1) * 128],
                                                         ident_b[:]), reads=[xn[i], ident_b], writes=[pb])
                    for kk in range(nk):
                        k = kb + kk
                        P.op("dve", lambda e: e.tensor_scalar(out=h[:, k * 512 + t4 * 128:k * 512 + (t4 + 1) * 128],
                                                              in0=pb[:, kk * 128:(kk + 1) * 128],
                                                              scalar1=s1c[:, k:k + 1], scalar2=modT[:, k:k + 1],
                                                              op0=ALU.mult, op1=ALU.add),
                             reads=[pb, s1c, modT], writes=[h])
            col0 = jc * TOK + st * 512
            for cc in ccs:
                if cc >= 3 * NHP:
                    j = cc - 3 * NHP
                    wt, wofs = wu, j * DC * 128
                else:
                    wt = wq[cnt["w"] % 3]
                    cnt["w"] += 1
                    wofs = 0
                    P.dma("pool", wt[:], w_in_l[cc, :, :], reads=[w_in_l], writes=[wt])
                pp = P.next_ps()
                for k in range(DC):
                    P.op("pe", lambda e: e.matmul(pp[:, :], lhsT=wt[:, wofs + k * 128:wofs + (k + 1) * 128],
                                                  rhs=h[:, k * 512:(k + 1) * 512], start=(k == 0), stop=(k == DC - 1)),
                         reads=[wt, h], writes=[pp])
                sg_ = stg[cnt["stg"] % 4]
                cnt["stg"] += 1
                if cc >= 3 * NHP:
                    P.op("act", lambda e: e.activation(out=sg_[:], in_=pp[:, :], func=AF.Identity,
                                                       scale=valid[:, jc:jc + 1]), reads=[pp, valid], writes=[sg_])
                else:
                    P.op("act", lambda e: e.activation(out=sg_[:], in_=pp[:, :], func=AF.Identity),
                         reads=[pp], writes=[sg_])
                P.dma("act", PT[cc * 128:(cc + 1) * 128, col0:col0 + 512], sg_[:], reads=[sg_], writes=[PTk[cc]])
    P.pop()

    P.push()
    ymixT = P.sb([128, (NHP + WC) * TOK], BF16, "ymixT")

    P.push()
    HO = (NCH - 2) * TOK
    mfull = P.sb([128, 256], BF16, "mfull")
    mfirst = P.sb([128, 256], BF16, "mfirst")
    P.dma("pool", mfull[:], mfull_in[:, :], reads=[mfull_in], writes=[mfull])
    P.dma("pool", mfirst[:], mfirst_in[:, :], reads=[mfirst_in], writes=[mfirst])
    agc = P.sb([128, NHP], F32, "agc")
    P.dma("sp", agc[:], ag_col[:, :], reads=[ag_col], writes=[agc])
    kb_index = {}
    for b, d in enumerate(BRANCH_DIL):
        span = 128 * d
        for n in range(-1, TOK // span):
            for r in range(d):
                kb_index[(b, n, r)] = len(kb_index)
    NKB = len(kb_index)
    QT = P.sb([128, TOK], BF16, "QT")
    KT = P.sb([128, 2 * TOK], BF16, "KT")
    VT = P.sb([128, 2 * TOK], BF16, "VT")
    VB = P.sb([128, NKB * 128], BF16, "VB")
    acc = P.sb([128, 2 * TOK], F32, "acc")
    ptile = [P.sb([128, 256], BF16, "ptile") for _ in range(4)]
    ptm = [P.sb([128, 256], BF16, "ptm") for _ in range(4)]
    ysq = P.sb([128, TOK], F32, "ysq")
    ssa = P.sb([128, NTT], F32, "ssa")
    sst = P.sb([128, NTT], F32, "sst")
    pcnt = 0
    blk_cnt = 0
    psH = []
    for b_ in P.psF:
        psH.append(Buf(b_.t[:, 0:256]))
        psH.append(Buf(b_.t[:, 256:512]))
    for hp in range(NHP):
        P.dma("sp", QT[:], PT[hp * 128:(hp + 1) * 128, HO + TOK:HO + 2 * TOK], reads=[PTk[hp]], writes=[QT])
        P.dma("sp", KT[:], PT[(NHP + hp) * 128:(NHP + hp + 1) * 128, HO:HO + 2 * TOK], reads=[PTk[NHP + hp]], writes=[KT])
        P.dma("sp", VT[:], PT[(2 * NHP + hp) * 128:(2 * NHP + hp + 1) * 128, HO:HO + 2 * TOK], reads=[PTk[2 * NHP + hp]], writes=[VT])

        def kpos(b, n, r):
            d = BRANCH_DIL[b]
            base = TOK + n * 128 * d + r
            return base, d

        items = list(kb_index.items())
        for g0 in range(0, NKB, 8):
            pb = psB[(g0 // 8) % 2]
            grp = items[g0:g0 + 8]
            for ii, ((b, n, r), idx) in enumerate(grp):
                base, d = kpos(b, n, r)
                P.op("pe", lambda e: e.transpose(pb[:, ii * 128:(ii + 1) * 128],
                                                 VT[:, base:base + 127 * d + 1:d], ident_b[:]),
                     reads=[VT, ident_b], writes=[pb])
            ng = len(grp)
            P.op("act", lambda e: e.activation(out=VB[:, g0 * 128:(g0 + ng) * 128], in_=pb[:, 0:ng * 128],
                                               func=AF.Identity), reads=[pb], writes=[VB])
        for b, d in enumerate(BRANCH_DIL):
            span = 128 * d
            for n in range(TOK // span):
                for r in range(d):
                    cb, _ = kpos(b, n, r)
                    pv, _ = kpos(b, n - 1, r)
                    qb = n * span + r
                    ci = kb_index[(b, n, r)]
                    pi = kb_index[(b, n - 1, r)]
                    hb_ = blk_cnt % 4
                    O = psH[8 + blk_cnt % 3]
                    blk_cnt += 1
                    for a in range(2):
                        lo, hi = a * 64, (a + 1) * 64
                        S = psH[4 * a + hb_]
                        P.op("pe", lambda e: e.matmul(S[:, 0:128], lhsT=KT[lo:hi, cb:cb + 127 * d + 1:d],
                                                      rhs=QT[lo:hi, qb:qb + 127 * d + 1:d], start=True, stop=True),
                             reads=[KT, QT], writes=[S])
                        P.op("pe", lambda e: e.matmul(S[:, 128:256], lhsT=KT[lo:hi, pv:pv + 127 * d + 1:d],
                                                      rhs=QT[lo:hi, qb:qb + 127 * d + 1:d], start=True, stop=True),
                             reads=[KT, QT], writes=[S])
                        pt = ptile[pcnt % 4]
                        pm = ptm[pcnt % 4]
                        pcnt += 1
                        P.op("act", lambda e: e.activation(out=pt[:], in_=S[:, 0:256], func=AF.Exp,
                                                           scale=HEAD_DIM ** -0.5), reads=[S], writes=[pt])
                        mk = mfirst if n == 0 else mfull
                        P.op("dve", lambda e: e.tensor_tensor(out=pm[:], in0=pt[:], in1=mk[:], op=ALU.mult),
                             reads=[pt, mk], writes=[pm])
                        P.op("pe", lambda e: e.matmul(O[lo:hi, 0:128], lhsT=VB[:, ci * 128 + lo:ci * 128 + hi],
                                                      rhs=pm[:, 0:128], start=True, stop=False), reads=[VB, pm], writes=[O])
                        P.op("pe", lambda e: e.matmul(O[lo:hi, 0:128], lhsT=VB[:, pi * 128 + lo:pi * 128 + hi],
                                                      rhs=pm[:, 128:256], start=False, stop=True), reads=[VB, pm], writes=[O])
                        P.op("pe", lambda e: e.matmul(O[lo:hi, 128:256], lhsT=ones_b[:, 0:64],
                                                      rhs=pm[:, 0:128], start=True, stop=False), reads=[ones_b, pm], writes=[O])
                        P.op("pe", lambda e: e.matmul(O[lo:hi, 128:256], lhsT=ones_b[:, 0:64],
                                                      rhs=pm[:, 128:256], start=False, stop=True), reads=[ones_b, pm], writes=[O])
                    av = acc[:, :].rearrange("p (c t) -> p c t", c=2)[:, :, qb:qb + 127 * d + 1:d]
                    ov = O[:, :].rearrange("p (c t) -> p c t", c=2)
                    if b == 0:
                        P.op("dve", lambda e: e.tensor_copy(out=av, in_=ov), reads=[O], writes=[acc])
                    else:
                        P.op("dve", lambda e: e.tensor_tensor(out=av, in0=ov, in1=av, op=ALU.add),
                             reads=[O, acc], writes=[acc])
        P.op("dve", lambda e: e.reciprocal(out=acc[:, TOK:2 * TOK], in_=acc[:, TOK:2 * TOK]), reads=[acc], writes=[acc])
        P.op("dve", lambda e: e.tensor_tensor(out=acc[:, 0:TOK], in0=acc[:, 0:TOK], in1=acc[:, TOK:2 * TOK], op=ALU.mult),
             reads=[acc], writes=[acc])
        if cfg.debug and hp < 4:
            P.dma("sp", DBG[:, hp * TOK:(hp + 1) * TOK], acc[:, 0:TOK], reads=[acc], writes=[DBG])
        P.op("pool", lambda e: e.tensor_tensor(out=ysq[:], in0=acc[:, 0:TOK], in1=acc[:, 0:TOK], op=ALU.mult),
             reads=[acc], writes=[ysq])
        sp_ = psH[11]
        for tt in range(NTT):
            P.op("pe", lambda e: e.matmul(sp_[:, tt:tt + 1], lhsT=ysq[:, tt * 128:(tt + 1) * 128], rhs=ones_f[:, 0:1],
                                          start=True, stop=True), reads=[ysq, ones_f], writes=[sp_])
        if hp == 0:
            P.op("dve", lambda e: e.tensor_copy(out=ssa[:], in_=sp_[:, 0:NTT]), reads=[sp_], writes=[ssa])
        else:
            P.op("dve", lambda e: e.tensor_tensor(out=ssa[:], in0=sp_[:, 0:NTT], in1=ssa[:], op=ALU.add),
                 reads=[sp_, ssa], writes=[ssa])
        P.op("act", lambda e: e.activation(out=ymixT[:, hp * TOK:(hp + 1) * TOK], in_=acc[:, 0:TOK], func=AF.Identity,
                                           scale=agc[:, hp:hp + 1]), reads=[acc, agc], writes=[ymixT])
    rstd_from_ss(ssa, ssa[:], A, rstd_a, rstd_a[:], width=NTT)
    P.pop()

    P.push()
    lre = P.sb([128, NT], F32, "lre")
    lim = P.sb([128, NT], F32, "lim")
    dtt = P.sb([128, NT], F32, "dtt")
    P.dma("sp", lre[:], lam_re_l[:, :], reads=[lam_re_l], writes=[lre])
    P.dma("sp", lim[:], lam_im_l[:, :], reads=[lam_im_l], writes=[lim])
    P.dma("sp", dtt[:], logdt_l[:, :], reads=[logdt_l], writes=[dtt])
    rho = P.sb([128, NT], F32, "rho")
    th = P.sb([128, NT], F32, "th")
    t0 = P.sb([128, NT], F32, "t0")
    t1 = P.sb([128, NT], F32, "t1")
    ti = P.sb([128, NT], I32, "ti")
    ck = P.sb([128, 12 * NT], F32, "ck")
    sk = P.sb([128, 12 * NT], F32, "sk")
    fre = P.sb([128, NT], F32, "fre")
    fim = P.sb([128, NT], F32, "fim")

    def tt_(out, a, b, op, rd, wr, eng="dve"):
        P.op(eng, lambda e: e.tensor_tensor(out=out, in0=a, in1=b, op=op), reads=rd, writes=wr)

    def ts_(out, a, s1, s2, op0, op1, rd, wr, eng="dve"):
        if s2 is None:
            P.op(eng, lambda e: e.tensor_scalar(out=out, in0=a, scalar1=s1, scalar2=None, op0=op0), reads=rd, writes=wr)
        else:
            P.op(eng, lambda e: e.tensor_scalar(out=out, in0=a, scalar1=s1, scalar2=s2, op0=op0, op1=op1), reads=rd, writes=wr)

    P.op("act", lambda e: e.activation(out=dtt[:], in_=dtt[:], func=AF.Exp), reads=[dtt], writes=[dtt])
    tt_(rho[:], lre[:], dtt[:], ALU.mult, [lre, dtt], [rho])
    P.op("act", lambda e: e.activation(out=rho[:], in_=rho[:], func=AF.Exp), reads=[rho], writes=[rho])
    tt_(th[:], lim[:], dtt[:], ALU.mult, [lim, dtt], [th])

    t2 = P.sb([128, NT], F32, "t2")

    def frac_pm_half(dstb, srcb):
        P.op("dve", lambda e: e.tensor_copy(out=ti[:], in_=srcb[:]), reads=[srcb], writes=[ti])
        P.op("dve", lambda e: e.tensor_copy(out=dstb[:], in_=ti[:]), reads=[ti], writes=[dstb])
        tt_(dstb[:], srcb[:], dstb[:], ALU.subtract, [srcb, dstb], [dstb])
        P.op("dve", lambda e: e.scalar_tensor_tensor(out=srcb[:], in0=dstb[:], scalar=0.5, in1=dstb[:], op0=ALU.is_gt,
                                                     op1=ALU.subtract), reads=[dstb], writes=[srcb])
        ts_(dstb[:], srcb[:], -1.0, None, ALU.mult, None, [srcb], [dstb])
        P.op("dve", lambda e: e.scalar_tensor_tensor(out=srcb[:], in0=dstb[:], scalar=-0.5, in1=dstb[:], op0=ALU.is_lt,
                                                     op1=ALU.add), reads=[dstb], writes=[srcb])
        P.op("dve", lambda e: e.tensor_copy(out=dstb[:], in_=srcb[:]), reads=[srcb], writes=[dstb])

    ts_(t0[:], th[:], 1.0 / (2 * math.pi), None, ALU.mult, None, [th], [t0])
    frac_pm_half(t2, t0)
    P.op("act", lambda e: e.activation(out=sk[:, 0:NT], in_=t2[:], func=AF.Sin, scale=2 * math.pi), reads=[t2], writes=[sk])
    ts_(t1[:], th[:], 1.0 / (2 * math.pi), 0.25, ALU.mult, ALU.add, [th], [t1])
    frac_pm_half(t2, t1)
    P.op("act", lambda e: e.activation(out=ck[:, 0:NT], in_=t2[:], func=AF.Sin, scale=2 * math.pi), reads=[t2], writes=[ck])
    for k in range(11):
        c0, s0 = ck[:, k * NT:(k + 1) * NT], sk[:, k * NT:(k + 1) * NT]
        c1, s1_ = ck[:, (k + 1) * NT:(k + 2) * NT], sk[:, (k + 1) * NT:(k + 2) * NT]
        tt_(t0[:], c0, c0, ALU.mult, [ck], [t0])
        tt_(t1[:], s0, s0, ALU.mult, [sk], [t1])
        tt_(c1, t0[:], t1[:], ALU.subtract, [t0, t1], [ck])
        tt_(t0[:], c0, s0, ALU.mult, [ck, sk], [t0])
        ts_(s1_, t0[:], 2.0, None, ALU.mult, None, [t0], [sk])
    abr = P.sb([128, NT], F32, "abr")
    abi = P.sb([128, NT], F32, "abi")
    den = P.sb([128, NT], F32, "den")
    tt_(abr[:], rho[:], ck[:, 0:NT], ALU.mult, [rho, ck], [abr])
    ts_(abr[:], abr[:], -1.0, None, ALU.add, None, [abr], [abr])
    tt_(abi[:], rho[:], sk[:, 0:NT], ALU.mult, [rho, sk], [abi])
    tt_(t0[:], lre[:], lre[:], ALU.mult, [lre], [t0])
    tt_(t1[:], lim[:], lim[:], ALU.mult, [lim], [t1])
    tt_(den[:], t0[:], t1[:], ALU.add, [t0, t1], [den])
    P.op("dve", lambda e: e.reciprocal(out=den[:], in_=den[:]), reads=[den], writes=[den])
    tt_(t0[:], abr[:], lre[:], ALU.mult, [abr, lre], [t0])
    tt_(t1[:], abi[:], lim[:], ALU.mult, [abi, lim], [t1])
    tt_(t0[:], t0[:], t1[:], ALU.add, [t0, t1], [t0])
    tt_(fre[:], t0[:], den[:], ALU.mult, [t0, den], [fre])
    tt_(t0[:], abi[:], lre[:], ALU.mult, [abi, lre], [t0])
    tt_(t1[:], abr[:], lim[:], ALU.mult, [abr, lim], [t1])
    tt_(t0[:], t0[:], t1[:], ALU.subtract, [t0, t1], [t0])
    tt_(fim[:], t0[:], den[:], ALU.mult, [t0, den], [fim])

    Bre = P.sb([128, NT * 128], BF16, "Bre")
    Bim = P.sb([128, NT * 128], BF16, "Bim")
    P.dma("pool", Bre[:], bre_l[:, :], reads=[bre_l], writes=[Bre], max_dma_last_dim=4096)
    P.dma("pool", Bim[:], bim_l[:, :], reads=[bim_l], writes=[Bim], max_dma_last_dim=4096)
    Cr = P.sb([128, NT * 128], BF16, "Cr")
    Ci = P.sb([128, NT * 128], BF16, "Ci")
    P.push()
    Craw_r = P.sb([128, NT * 128], F32, "Crr")
    Craw_i = P.sb([128, NT * 128], F32, "Cri")
    P.dma("sp", Craw_r[:], cre_l[:, :], reads=[cre_l], writes=[Craw_r])
    P.dma("sp", Craw_i[:], cim_l[:, :], reads=[cim_l], writes=[Craw_i])
    ctmp = P.sb([128, 128], F32, "ctmp")
    for j in range(NT):
        sl = slice(j * 128, (j + 1) * 128)
        ts_(ctmp[:], Craw_i[:, sl], fim[:, j:j + 1], None, ALU.mult, None, [Craw_i, fim], [ctmp])
        P.op("dve", lambda e: e.scalar_tensor_tensor(out=Cr[:, sl], in0=Craw_r[:, sl], scalar=fre[:, j:j + 1], in1=ctmp[:],
                                                     op0=ALU.mult, op1=ALU.subtract), reads=[Craw_r, fre, ctmp], writes=[Cr])
        ts_(ctmp[:], Craw_i[:, sl], fre[:, j:j + 1], None, ALU.mult, None, [Craw_i, fre], [ctmp])
        P.op("dve", lambda e: e.scalar_tensor_tensor(out=Ci[:, sl], in0=Craw_r[:, sl], scalar=fim[:, j:j + 1], in1=ctmp[:],
                                                     op0=ALU.mult, op1=ALU.add), reads=[Craw_r, fim, ctmp], writes=[Ci])
        ts_(Ci[:, sl], Ci[:, sl], -1.0, None, ALU.mult, None, [Ci], [Ci])
    P.pop()
    dsk = P.sb([128, WC], F32, "dsk")
    P.dma("sp", dsk[:], dskip_col[:, :], reads=[dskip_col], writes=[dsk])

    kbig = int(round(math.log2(TOK)))
    NTH = TOK // 128
    rk = P.sb([128, 12 * NT], F32, "rk")
    ark = P.sb([128, 12 * NT], F32, "ark")
    aik = P.sb([128, 12 * NT], F32, "aik")
    P.op("dve", lambda e: e.tensor_copy(out=rk[:, 0:NT], in_=rho[:]), reads=[rho], writes=[rk])
    for k in range(11):
        tt_(rk[:, (k + 1) * NT:(k + 2) * NT], rk[:, k * NT:(k + 1) * NT], rk[:, k * NT:(k + 1) * NT], ALU.mult, [rk], [rk])
    tt_(ark[:], rk[:], ck[:], ALU.mult, [rk, ck], [ark])
    tt_(aik[:], rk[:], sk[:], ALU.mult, [rk, sk], [aik])
    Vst = P.sb([128, 2 * NT], F32, "Vst")
    P.op("dve", lambda e: e.memset(Vst[:], 0.0), writes=[Vst])
    P.push()
    Pr = P.sb([128, TOK], F32, "Pr")
    Pi = P.sb([128, TOK], F32, "Pi")
    ptmp = P.sb([128, TOK // 2], F32, "ptmp")
    PTt = [P.sb([128, 2 * TOK], BF16, "PTt") for _ in range(4)]
    B2 = [P.sb([128, 256], F32, "B2") for _ in range(4)]
    UTp = [P.sb([128, TOK], BF16, "UTp") for _ in range(2)]
    UTT = [P.sb([128, TOK], BF16, "UTT") for _ in range(2)]
    sacc = P.sb([128, 4], F32, "sacc")
    hv = P.sb([128, 6], F32, "hv")
    junkS = P.sb([128, 128], F32, "junkS")
    pcn = 0
    for chq in range(WC):
        for jj in range(4):
            j = chq * 4 + jj
            P.op("dve", lambda e: e.memset(Pr[:, TOK - 1:TOK], 1.0), writes=[Pr])
            P.op("dve", lambda e: e.memset(Pi[:, TOK - 1:TOK], 0.0), writes=[Pi])
            k = 0
            w = 1
            while w < TOK:
                s_ = slice(TOK - w, TOK)
                d_ = slice(TOK - 2 * w, TOK - w)
                aR = ark[:, k * NT + j:k * NT + j + 1]
                aI = aik[:, k * NT + j:k * NT + j + 1]
                ts_(ptmp[:, 0:w], Pi[:, s_], aI, None, ALU.mult, None, [Pi, aik], [ptmp])
                P.op("dve", lambda e: e.scalar_tensor_tensor(out=Pr[:, d_], in0=Pr[:, s_], scalar=aR, in1=ptmp[:, 0:w],
                                                             op0=ALU.mult, op1=ALU.subtract), reads=[Pr, ark, ptmp], writes=[Pr])
                ts_(ptmp[:, 0:w], Pr[:, s_], aI, None, ALU.mult, None, [Pr, aik], [ptmp])
                P.op("dve", lambda e: e.scalar_tensor_tensor(out=Pi[:, d_], in0=Pi[:, s_], scalar=aR, in1=ptmp[:, 0:w],
                                                             op0=ALU.mult, op1=ALU.add), reads=[Pi, aik, ptmp], writes=[Pi])
                w *= 2
                k += 1
            for hh, Psrc in enumerate((Pr, Pi)):
                for t0_ in range(0, NTH, 4):
                    tp = P.next_ps()
                    for q4 in range(4):
                        th_ = t0_ + q4
                        P.op("pe", lambda e: e.transpose(tp[:, q4 * 128:(q4 + 1) * 128], Psrc[:, th_ * 128:(th_ + 1) * 128], ident_f[:]),
                             reads=[Psrc, ident_f], writes=[tp])
                    P.op("act", lambda e: e.activation(out=PTt[jj][:, hh * TOK + t0_ * 128:hh * TOK + (t0_ + 4) * 128], in_=tp[:, :],
                                                       func=AF.Identity), reads=[tp], writes=[PTt[jj]])
            tb = psB[pcn % 2]
            pcn += 1
            P.op("pe", lambda e: e.transpose(tb[:, 0:128], Bre[:, j * 128:(j + 1) * 128], ident_b[:]), reads=[Bre, ident_b], writes=[tb])
            P.op("pe", lambda e: e.transpose(tb[:, 128:256], Bim[:, j * 128:(j + 1) * 128], ident_b[:]), reads=[Bim, ident_b], writes=[tb])
            P.op("act", lambda e: e.activation(out=B2[jj][:], in_=tb[:, 0:256], func=AF.Identity), reads=[tb], writes=[B2[jj]])
        for jc in range(NCH - 1):
            u = UTp[jc % 2]
            utt = UTT[jc % 2]
            urow = (3 * NHP + chq) * 128
            P.dma("sp", u[:], PT[urow:urow + 128, jc * TOK:(jc + 1) * TOK], reads=[PTk[3 * NHP + chq]], writes=[u])
            for g0 in range(0, NTH, 8):
                tb = psB[pcn % 2]
                pcn += 1
                for q8 in range(8):
                    th_ = g0 + q8
                    P.op("pe", lambda e: e.transpose(tb[:, q8 * 128:(q8 + 1) * 128], u[:, th_ * 128:(th_ + 1) * 128], ident_b[:]),
                         reads=[u, ident_b], writes=[tb])
                P.op("act", lambda e: e.activation(out=utt[:, g0 * 128:(g0 + 8) * 128], in_=tb[:, :], func=AF.Identity),
                     reads=[tb], writes=[utt])
            for jj in range(4):
                j = chq * 4 + jj
                mp = P.next_ps()
                for hh in range(2):
                    for th_ in range(NTH):
                        P.op("pe", lambda e: e.matmul(mp[:, hh * 128:(hh + 1) * 128],
                                                      lhsT=PTt[jj][:, hh * TOK + th_ * 128:hh * TOK + (th_ + 1) * 128],
                                                      rhs=utt[:, th_ * 128:(th_ + 1) * 128], start=(th_ == 0), stop=(th_ == NTH - 1)),
                             reads=[PTt[jj], utt], writes=[mp])
                combos = ((0, 0), (1, 1), (1, 0), (0, 1))
                for ci_, (mh, bh) in enumerate(combos):
                    P.op("dve", lambda e: e.scalar_tensor_tensor(out=junkS[:], in0=mp[:, mh * 128:(mh + 1) * 128], scalar=1.0,
                                                                 in1=B2[jj][:, bh * 128:(bh + 1) * 128], op0=ALU.mult, op1=ALU.mult,
                                                                 accum_out=sacc[:, ci_:ci_ + 1]), reads=[mp, B2[jj]], writes=[junkS, sacc])
                aR = ark[:, kbig * NT + j:kbig * NT + j + 1]
                aI = aik[:, kbig * NT + j:kbig * NT + j + 1]
                Vr = Vst[:, j:j + 1]
                Vi = Vst[:, NT + j:NT + j + 1]
                tt_(hv[:, 4:5], sacc[:, 0:1], sacc[:, 1:2], ALU.subtract, [sacc], [hv])
                tt_(hv[:, 5:6], sacc[:, 2:3], sacc[:, 3:4], ALU.add, [sacc], [hv])
                ts_(hv[:, 0:1], Vi, aI, None, ALU.mult, None, [Vst, aik], [hv])
                P.op("dve", lambda e: e.scalar_tensor_tensor(out=hv[:, 1:2], in0=Vr, scalar=aR, in1=hv[:, 0:1],
                                                             op0=ALU.mult, op1=ALU.subtract), reads=[Vst, ark, hv], writes=[hv])
                ts_(hv[:, 2:3], Vr, aI, None, ALU.mult, None, [Vst, aik], [hv])
                P.op("dve", lambda e: e.scalar_tensor_tensor(out=hv[:, 3:4], in0=Vi, scalar=aR, in1=hv[:, 2:3],
                                                             op0=ALU.mult, op1=ALU.add), reads=[Vst, ark, hv], writes=[hv])
                tt_(Vr, hv[:, 1:2], hv[:, 4:5], ALU.add, [hv], [Vst])
                tt_(Vi, hv[:, 3:4], hv[:, 5:6], ALU.add, [hv], [Vst])
    P.pop()

    P.push()
    ct = P.sb([128, TOK], F32, "ct")
    st_ = P.sb([128, TOK], F32, "st")
    UT = [P.sb([128, TOK], BF16, "UT") for _ in range(2)]
    brs = [P.sb([128, 512], F32, "brs") for _ in range(2)]
    bis = [P.sb([128, 512], F32, "bis") for _ in range(2)]
    wr_ = P.sb([128, TOK], F32, "wr")
    wi_ = P.sb([128, TOK], F32, "wi")
    tmpA = wr_
    gq = wi_
    zr = P.sb([128, TOK], F32, "zr")
    zi = P.sb([128, TOK], F32, "zi")
    pa = [P.sb([128, 512], F32, "pa") for _ in range(2)]
    pb_ = [P.sb([128, 512], F32, "pbb") for _ in range(2)]
    da = [P.sb([128, 512], F32, "da") for _ in range(2)]
    db = [P.sb([128, 512], F32, "db") for _ in range(2)]
    xrb = [P.sb([128, 512], BF16, "xr") for _ in range(2)]
    xib = [P.sb([128, 512], BF16, "xi") for _ in range(2)]
    zc = P.sb([128, 4], F32, "zc")
    yT = P.sb([128, TOK], F32, "yT")
    YG0 = NHP * TOK
    ucnt = 0
    for j in range(NT):
        chq = j // 4
        P.op("dve", lambda e: e.memset(ct[:, 0:1], 1.0), writes=[ct])
        P.op("dve", lambda e: e.memset(st_[:, 0:1], 0.0), writes=[st_])
        k = 0
        w = 1
        while w < TOK:
            cK = ck[:, k * NT + j:k * NT + j + 1]
            sK = sk[:, k * NT + j:k * NT + j + 1]
            ts_(tmpA[:, 0:w], st_[:, 0:w], sK, None, ALU.mult, None, [st_, sk], [tmpA])
            P.op("dve", lambda e: e.scalar_tensor_tensor(out=ct[:, w:2 * w], in0=ct[:, 0:w], scalar=cK, in1=tmpA[:, 0:w],
                                                         op0=ALU.mult, op1=ALU.subtract), reads=[ct, ck, tmpA], writes=[ct])
            ts_(tmpA[:, 0:w], ct[:, 0:w], sK, None, ALU.mult, None, [ct, sk], [tmpA])
            P.op("dve", lambda e: e.scalar_tensor_tensor(out=st_[:, w:2 * w], in0=st_[:, 0:w], scalar=cK, in1=tmpA[:, 0:w],
                                                         op0=ALU.mult, op1=ALU.add), reads=[st_, ck, tmpA], writes=[st_])
            w *= 2
            k += 1
        c0 = ck[:, j:j + 1]
        s0 = sk[:, j:j + 1]
        Vr = Vst[:, j:j + 1]
        Vi = Vst[:, NT + j:NT + j + 1]
        ts_(zc[:, 2:3], Vi, s0, None, ALU.mult, None, [Vst, sk], [zc])
        P.op("dve", lambda e: e.scalar_tensor_tensor(out=zc[:, 0:1], in0=Vr, scalar=c0, in1=zc[:, 2:3],
                                                     op0=ALU.mult, op1=ALU.subtract), reads=[Vst, ck, zc], writes=[zc])
        ts_(zc[:, 3:4], Vr, s0, None, ALU.mult, None, [Vst, sk], [zc])
        P.op("dve", lambda e: e.scalar_tensor_tensor(out=zc[:, 1:2], in0=Vi, scalar=c0, in1=zc[:, 3:4],
                                                     op0=ALU.mult, op1=ALU.add), reads=[Vst, ck, zc], writes=[zc])
        for jc in (NCH - 1,):
            u = UT[ucnt % 2]
            ucnt += 1
            urow = (3 * NHP + chq) * 128
            P.dma("sp", u[:], PT[urow:urow + 128, jc * TOK:(jc + 1) * TOK], reads=[PTk[3 * NHP + chq]], writes=[u])
            for g in range(TOK // 512):
                gs = slice(g * 512, (g + 1) * 512)
                pr = P.next_ps()
                pi_ = P.next_ps()
                P.op("pe", lambda e: e.matmul(pr[:, :], lhsT=Bre[:, j * 128:(j + 1) * 128], rhs=u[:, gs], start=True, stop=True),
                     reads=[Bre, u], writes=[pr])
                P.op("pe", lambda e: e.matmul(pi_[:, :], lhsT=Bim[:, j * 128:(j + 1) * 128], rhs=u[:, gs], start=True, stop=True),
                     reads=[Bim, u], writes=[pi_])
                b_r = brs[g % 2]
                b_i = bis[g % 2]
                P.op("act", lambda e: e.activation(out=b_r[:], in_=pr[:, :], func=AF.Identity), reads=[pr], writes=[b_r])
                P.op("act", lambda e: e.activation(out=b_i[:], in_=pi_[:, :], func=AF.Identity), reads=[pi_], writes=[b_i])
                A_, B_ = pa[g % 2], pb_[g % 2]
                tt_(A_[:], b_r[:], ct[:, gs], ALU.mult, [b_r, ct], [A_])
                tt_(B_[:], b_i[:], st_[:, gs], ALU.mult, [b_i, st_], [B_])
                tt_(wr_[:, gs], A_[:], B_[:], ALU.add, [A_, B_], [wr_])
                C_, D_ = da[g % 2], db[g % 2]
                tt_(C_[:], b_i[:], ct[:, gs], ALU.mult, [b_i, ct], [C_])
                tt_(D_[:], b_r[:], st_[:, gs], ALU.mult, [b_r, st_], [D_])
                tt_(wi_[:, gs], C_[:], D_[:], ALU.subtract, [C_, D_], [wi_], eng="pool")
            rb = rho[:, j:j + 1].to_broadcast([128, TOK])
            P.op("dve", lambda e: e.tensor_tensor_scan(out=zr[:], data0=rb, data1=wr_[:], initial=zc[:, 0:1],
                                                       op0=ALU.mult, op1=ALU.add), reads=[rho, wr_, zc], writes=[zr])
            P.op("dve", lambda e: e.tensor_tensor_scan(out=zi[:], data0=rb, data1=wi_[:], initial=zc[:, 1:2],
                                                       op0=ALU.mult, op1=ALU.add), reads=[rho, wi_, zc], writes=[zi])
            if True:
                for g in range(TOK // 512):
                    gs = slice(g * 512, (g + 1) * 512)
                    A_, B_ = pa[g % 2], pb_[g % 2]
                    tt_(A_[:], zr[:, gs], ct[:, gs], ALU.mult, [zr, ct], [A_])
                    tt_(B_[:], zi[:, gs], st_[:, gs], ALU.mult, [zi, st_], [B_])
                    xr = xrb[g % 2]
                    nxi = xib[g % 2]
                    tt_(xr[:], A_[:], B_[:], ALU.subtract, [A_, B_], [xr])
                    C_, D_ = da[g % 2], db[g % 2]
                    tt_(C_[:], zr[:, gs], st_[:, gs], ALU.mult, [zr, st_], [C_])
                    tt_(D_[:], zi[:, gs], ct[:, gs], ALU.mult, [zi, ct], [D_])
                    tt_(nxi[:], C_[:], D_[:], ALU.add, [C_, D_], [nxi], eng="pool")
                    yp = P.next_ps()
                    P.op("pe", lambda e: e.matmul(yp[:, :], lhsT=Cr[:, j * 128:(j + 1) * 128], rhs=xr[:], start=True, stop=False),
                         reads=[Cr, xr], writes=[yp])
                    P.op("pe", lambda e: e.matmul(yp[:, :], lhsT=Ci[:, j * 128:(j + 1) * 128], rhs=nxi[:], start=False, stop=True),
                         reads=[Ci, nxi], writes=[yp])
                    if j % 4 == 0:
                        P.op("dve", lambda e: e.scalar_tensor_tensor(out=yT[:, gs], in0=u[:, gs], scalar=dsk[:, chq:chq + 1],
                                                                     in1=yp[:, :], op0=ALU.mult, op1=ALU.add),
                             reads=[u, dsk, yp], writes=[yT])
                    else:
                        tt_(yT[:, gs], yp[:, :], yT[:, gs], ALU.add, [yp, yT], [yT])
        if j % 4 == 3:
            if cfg.debug and chq < 2:
                P.dma("sp", DBG[:, (6 + chq) * TOK:(7 + chq) * TOK], yT[:], reads=[yT], writes=[DBG])
            tt_(gq[:], yT[:], yT[:], ALU.mult, [yT], [gq], eng="pool")
            ts_(gq[:], gq[:], 0.044715, 1.0, ALU.mult, ALU.add, [gq], [gq], eng="pool")
            tt_(gq[:], gq[:], yT[:], ALU.mult, [gq, yT], [gq], eng="pool")
            P.op("act", lambda e: e.activation(out=gq[:], in_=gq[:], func=AF.Sigmoid, scale=2.0 * math.sqrt(2.0 / math.pi)),
                 reads=[gq], writes=[gq])
            tt_(ymixT[:, YG0 + chq * TOK:YG0 + (chq + 1) * TOK], gq[:], yT[:], ALU.mult, [gq, yT], [ymixT], eng="pool")
    P.pop()
    wgl = P.sb([128, WC * W], BF16, "wgl")
    P.dma("pool", wgl[:], w_glu_l[:, :], reads=[w_glu_l], writes=[wgl], max_dma_last_dim=4096)
    bgl = P.sb([128, WC], F32, "bgl")
    sgc = P.sb([128, WC], F32, "sgc")
    P.dma("sp", bgl[:], b_glu_col[:, :], reads=[b_glu_col], writes=[bgl])
    P.dma("sp", sgc[:], sg_col[:, :], reads=[sg_col], writes=[sgc])
    sig = [P.sb([128, 512], F32, "sig") for _ in range(2)]
    ysg = [P.sb([128, 512], F32, "ysg") for _ in range(2)]
    ysq2 = [P.sb([128, 512], F32, "ysq2") for _ in range(2)]
    gtmp = P.sb([128, WC * 512], BF16, "gtmp")
    sss = P.sb([128, NTT], F32, "sss")
    for g in range(TOK // 512):
        for oc in range(WC):
            gp = P.next_ps()
            for k in range(WC):
                P.op("pe", lambda e: e.matmul(gp[:, :], lhsT=wgl[:, k * W + oc * 128:k * W + (oc + 1) * 128],
                                              rhs=ymixT[:, YG0 + k * TOK + g * 512:YG0 + k * TOK + (g + 1) * 512],
                                              start=(k == 0), stop=(k == WC - 1)), reads=[wgl, ymixT], writes=[gp])
            sgt = sig[oc % 2]
            yst = ysg[oc % 2]
            yq = ysq2[oc % 2]
            P.op("act", lambda e: e.activation(out=sgt[:], in_=gp[:, :], func=AF.Sigmoid, bias=bgl[:, oc:oc + 1]),
                 reads=[gp, bgl], writes=[sgt])
            tt_(yst[:], sgt[:], ymixT[:, YG0 + oc * TOK + g * 512:YG0 + oc * TOK + (g + 1) * 512], ALU.mult, [sgt, ymixT], [yst])
            if cfg.debug and oc < 2:
                P.dma("sp", DBG[:, (4 + oc) * TOK + g * 512:(4 + oc) * TOK + (g + 1) * 512], yst[:], reads=[yst], writes=[DBG])
            tt_(yq[:], yst[:], yst[:], ALU.mult, [yst], [yq], eng="pool")
            sp_ = P.next_ps()
            for t4 in range(4):
                P.op("pe", lambda e: e.matmul(sp_[:, t4:t4 + 1], lhsT=yq[:, t4 * 128:(t4 + 1) * 128], rhs=ones_f[:, 0:1],
                                              start=True, stop=True), reads=[yq, ones_f], writes=[sp_])
            if oc == 0:
                P.op("dve", lambda e: e.tensor_copy(out=sss[:, g * 4:(g + 1) * 4], in_=sp_[:, 0:4]), reads=[sp_], writes=[sss])
            else:
                tt_(sss[:, g * 4:(g + 1) * 4], sp_[:, 0:4], sss[:, g * 4:(g + 1) * 4], ALU.add, [sp_, sss], [sss])
            P.op("act", lambda e: e.activation(out=gtmp[:, oc * 512:(oc + 1) * 512], in_=yst[:], func=AF.Identity,
                                               scale=sgc[:, oc:oc + 1]), reads=[yst, sgc], writes=[gtmp])
        for oc in range(WC):
            P.op("pool", lambda e: e.tensor_copy(out=ymixT[:, YG0 + oc * TOK + g * 512:YG0 + oc * TOK + (g + 1) * 512],
                                                 in_=gtmp[:, oc * 512:(oc + 1) * 512]), reads=[gtmp], writes=[ymixT])
    rstd_from_ss(sss, sss[:], W, rstd_s, rstd_s[:], width=NTT)
    P.pop()

    P.push()
    NK = NHP + WC
    wo = P.sb([128, NK * D], BF16, "wo")
    for k in range(NK):
        P.dma("pool", wo[:, k * D:(k + 1) * D], w_out_l[:, k * D:(k + 1) * D], reads=[w_out_l], writes=[wo],
              max_dma_last_dim=4096)
    g1b = P.sb([128, D], F32, "g1b")
    P.dma("sp", g1b[:], modrow[2 * DC:3 * DC, :].rearrange("(o a) b -> o (a b)", o=1).partition_broadcast(128), reads=[modrow], writes=[g1b])
    wr_f = P.sb([128, DC * E], F32, "wrf")
    P.dma("sp", wr_f[:], w_r_l[:, :], reads=[w_r_l], writes=[wr_f])
    brb = P.sb([128, E], F32, "brb")
    P.dma("sp", brb[:], b_r_row[0:1, :].partition_broadcast(128), reads=[b_r_row], writes=[brb])
    xo = [P.sb([128, D], F32, "xo") for _ in range(2)]
    x2 = [P.sb([128, D], F32, "x2") for _ in range(2)]
    ta = [P.sb([128, 512], F32, "ta") for _ in range(2)]
    xn2 = P.sb([128, D], F32, "xn2")
    h2f = P.sb([128, DC * 128], F32, "h2f")
    h2b = [P.sb([128, DC * 128], BF16, "h2b") for _ in range(2)]
    ss2 = P.sb([128, 1], F32, "ss2")
    rs2 = P.sb([128, 1], F32, "rs2")
    lg = P.sb([128, E], F32, "lg")
    m8 = P.sb([128, 8], F32, "m8")
    nmx = P.sb([128, 1], F32, "nmx")
    msk = P.sb([128, E], F32, "msk")
    ex = P.sb([128, E], F32, "ex")
    dn = P.sb([128, 1], F32, "dn")
    OWN = (NCH - 1) * TOK
    for tt in range(NTT):
        i = tt % 2
        P.dma("sp", xo[i][:], x_ext[OWN + tt * 128:OWN + (tt + 1) * 128, :], reads=[x_ext], writes=[xo[i]])
        for cg in range(D // 512):
            cs = slice(cg * 512, (cg + 1) * 512)
            pA = P.next_ps()
            pS = P.next_ps()
            for k in range(NHP):
                P.op("pe", lambda e: e.matmul(pA[:, :], lhsT=ymixT[:, k * TOK + tt * 128:k * TOK + (tt + 1) * 128],
                                              rhs=wo[:, k * D + cg * 512:k * D + (cg + 1) * 512],
                                              start=(k == 0), stop=(k == NHP - 1)), reads=[ymixT, wo], writes=[pA])
            for k in range(NHP, NK):
                P.op("pe", lambda e: e.matmul(pS[:, :], lhsT=ymixT[:, k * TOK + tt * 128:k * TOK + (tt + 1) * 128],
                                              rhs=wo[:, k * D + cg * 512:k * D + (cg + 1) * 512],
                                              start=(k == NHP), stop=(k == NK - 1)), reads=[ymixT, wo], writes=[pS])
            t_ = ta[cg % 2]
            ts_(t_[:], pA[:, :], rstd_a[:, tt:tt + 1], None, ALU.mult, None, [pA, rstd_a], [t_])
            P.op("dve", lambda e: e.scalar_tensor_tensor(out=t_[:], in0=pS[:, :], scalar=rstd_s[:, tt:tt + 1], in1=t_[:],
                                                         op0=ALU.mult, op1=ALU.add), reads=[pS, rstd_s, t_], writes=[t_])
            tt_(t_[:], t_[:], g1b[:, cs], ALU.mult, [t_, g1b], [t_])
            tt_(x2[i][:, cs], t_[:], xo[i][:, cs], ALU.add, [t_, xo[i]], [x2[i]])
        P.dma("sp", X2[tt * 128:(tt + 1) * 128, :], x2[i][:], reads=[x2[i]], writes=[X2k[tt]])
        P.op("act", lambda e: e.activation(out=xn2[:], in_=x2[i][:], func=AF.Square, accum_out=ss2[:, 0:1]),
             reads=[x2[i]], writes=[xn2, ss2])
        rstd_from_ss(ss2, ss2[:, 0:1], D, rs2, rs2[:, 0:1])
        P.op("dve", lambda e: e.tensor_scalar(out=xn2[:], in0=x2[i][:], scalar1=rs2[:, 0:1], scalar2=None, op0=ALU.mult),
             reads=[x2[i], rs2], writes=[xn2])
        hb = h2b[i]
        for kb in range(0, DC, 4):
            tp = P.next_ps()
            nk = min(4, DC - kb)
            for kk in range(nk):
                k = kb + kk
                P.op("pe", lambda e: e.transpose(tp[:, kk * 128:(kk + 1) * 128], xn2[:, k * 128:(k + 1) * 128], ident_f[:]),
                     reads=[xn2, ident_f], writes=[tp])
            for kk in range(nk):
                k = kb + kk
                P.op("dve", lambda e: e.tensor_scalar(out=h2f[:, k * 128:(k + 1) * 128], in0=tp[:, kk * 128:(kk + 1) * 128],
                                                      scalar1=s2c[:, k:k + 1], scalar2=modT[:, 3 * DC + k:3 * DC + k + 1],
                                                      op0=ALU.mult, op1=ALU.add), reads=[tp, s2c, modT], writes=[h2f])
        P.op("act", lambda e: e.activation(out=hb[:], in_=h2f[:], func=AF.Identity), reads=[h2f], writes=[hb])
        P.dma("sp", H2T[:, tt * 128:(tt + 1) * 128].rearrange("(k p) t -> p k t", p=128),
              hb[:, :].rearrange("p (k t) -> p k t", k=DC), reads=[hb], writes=[H2T])
        lp = P.next_ps()
        for k in range(DC):
            P.op("pe", lambda e: e.matmul(lp[:, 0:E], lhsT=h2f[:, k * 128:(k + 1) * 128], rhs=wr_f[:, k * E:(k + 1) * E],
                                          start=(k == 0), stop=(k == DC - 1)), reads=[h2f, wr_f], writes=[lp])
        tt_(lg[:], lp[:, 0:E], brb[:], ALU.add, [lp, brb], [lg])
        P.op("dve", lambda e: e.max(out=m8[:], in_=lg[:]), reads=[lg], writes=[m8])
        ts_(msk[:], lg[:], m8[:, TOP_K - 1:TOP_K], None, ALU.is_ge, None, [lg, m8], [msk])
        ts_(nmx[:], m8[:, 0:1], -1.0, None, ALU.mult, None, [m8], [nmx])
        P.op("act", lambda e: e.activation(out=ex[:], in_=lg[:], func=AF.Exp, bias=nmx[:, 0:1]), reads=[lg, nmx], writes=[ex])
        tt_(ex[:], ex[:], msk[:], ALU.mult, [ex, msk], [ex])
        P.op("dve", lambda e: e.reduce_sum(out=dn[:], in_=ex[:], axis=AX.X), reads=[ex], writes=[dn])
        P.op("dve", lambda e: e.reciprocal(out=dn[:], in_=dn[:]), reads=[dn], writes=[dn])
        ts_(G[:, tt * E:(tt + 1) * E], ex[:], dn[:, 0:1], None, ALU.mult, None, [ex, dn], [G])
    if cfg.debug:
        GD = P.dram("GD", [128, NTT * E], F32, kind=SK)
        P.dma("sp", GD[:, :], G[:], reads=[G], writes=[GD])
    P.pop()
    P.pop()

    HALF = TOK // 2
    HT = HALF // 128
    NG = HALF // 512
    NCG = D // 512
    for hf in range(2):
        P.push()
        yacc = P.sb([128, HT * D], F32, "yacc")
        P.push()
        GT = P.sb([E, HALF], F32, "GT")
        bdn = P.sb([E, D], F32, "bdn")
        P.dma("sp", bdn[:], b_down[:, :], reads=[b_down], writes=[bdn])
        for t8 in range(HT):
            tg = hf * HT + t8
            tp = P.next_ps()
            P.op("pe", lambda e: e.transpose(tp[0:E, 0:128], G[:, tg * E:(tg + 1) * E], ident_f[:]),
                 reads=[G, ident_f], writes=[tp])
            P.op("dve", lambda e: e.tensor_copy(out=GT[:, t8 * 128:(t8 + 1) * 128], in_=tp[0:E, 0:128]),
                 reads=[tp], writes=[GT])
        for t8 in range(HT):
            for cg in range(NCG):
                bp = P.next_ps()
                P.op("pe", lambda e: e.matmul(bp[:, :], lhsT=GT[:, t8 * 128:(t8 + 1) * 128], rhs=bdn[:, cg * 512:(cg + 1) * 512],
                                              start=True, stop=True), reads=[GT, bdn], writes=[bp])
                P.op("act", lambda e: e.activation(out=yacc[:, t8 * D + cg * 512:t8 * D + (cg + 1) * 512], in_=bp[:, :],
                                                   func=AF.Identity), reads=[bp], writes=[yacc])
        P.pop()
        P.push()
        bgc = P.sb([128, E * FC], F32, "bgc")
        blc = P.sb([128, E * FC], F32, "blc")
        P.dma("sp", bgc[:], bg_col[:, :], reads=[bg_col], writes=[bgc])
        P.dma("sp", blc[:], bl_col[:, :], reads=[bl_col], writes=[blc])
        h2h = P.sb([128, DC * HALF], BF16, "h2h")
        actT = P.sb([128, FC * HALF], BF16, "actT")
        wg = [P.sb([128, DC * 128], BF16, "wg") for _ in range(2)]
        wl = [P.sb([128, DC * 128], BF16, "wl") for _ in range(2)]
        wd = [P.sb([128, FC * 512], BF16, "wd") for _ in range(2)]
        gclb = [P.sb([128, 512], F32, "gcl") for _ in range(2)]
        sgmb = [P.sb([128, 512], F32, "sgm") for _ in range(2)]
        lclb = [P.sb([128, 512], F32, "lcl") for _ in range(2)]
        ecnt = 0
        P.dma("sp", h2h[:, :].rearrange("p (k t) -> p k t", k=DC),
              H2T[:, hf * HALF:(hf + 1) * HALF].rearrange("(k p) t -> p k t", p=128), reads=[H2T], writes=[h2h])
        wc = 0
        dcn = 0
        for ex_ in range(E):
            for fc in range(FC):
                wgt = wg[wc % 2]
                wlt = wl[wc % 2]
                wc += 1
                P.dma("pool", wgt[:], w_gate_l[ex_ * FC + fc, :, :], reads=[w_gate_l], writes=[wgt])
                P.dma("pool", wlt[:], w_lin_l[ex_ * FC + fc, :, :], reads=[w_lin_l], writes=[wlt])
                col = ex_ * FC + fc
                for g in range(NG):
                    gp = P.next_ps()
                    lp = P.next_ps()
                    for k in range(DC):
                        P.op("pe", lambda e: e.matmul(gp[:, :], lhsT=wgt[:, k * 128:(k + 1) * 128],
                                                      rhs=h2h[:, k * HALF + g * 512:k * HALF + (g + 1) * 512],
                                                      start=(k == 0), stop=(k == DC - 1)), reads=[wgt, h2h], writes=[gp])
                    for k in range(DC):
                        P.op("pe", lambda e: e.matmul(lp[:, :], lhsT=wlt[:, k * 128:(k + 1) * 128],
                                                      rhs=h2h[:, k * HALF + g * 512:k * HALF + (g + 1) * 512],
                                                      start=(k == 0), stop=(k == DC - 1)), reads=[wlt, h2h], writes=[lp])
                    gcl, sgm, lcl = gclb[ecnt % 2], sgmb[ecnt % 2], lclb[ecnt % 2]
                    ecnt += 1
                    ts_(gcl[:], gp[:, :], bgc[:, col:col + 1], SW_LIMIT, ALU.add, ALU.min, [gp, bgc], [gcl])
                    P.op("act", lambda e: e.activation(out=sgm[:], in_=gcl[:], func=AF.Sigmoid, scale=SW_ALPHA),
                         reads=[gcl], writes=[sgm])
                    ts_(lcl[:], lp[:, :], blc[:, col:col + 1], SW_LIMIT, ALU.add, ALU.min, [lp, blc], [lcl])
                    ts_(lcl[:], lcl[:], -SW_LIMIT, 1.0, ALU.max, ALU.add, [lcl], [lcl])
                    tt_(sgm[:], gcl[:], sgm[:], ALU.mult, [gcl, sgm], [sgm])
                    tt_(actT[:, fc * HALF + g * 512:fc * HALF + (g + 1) * 512], sgm[:], lcl[:], ALU.mult,
                        [sgm, lcl], [actT])
            for cg in range(NCG):
                wdt = wd[dcn % 2]
                dcn += 1
                P.dma("pool", wdt[:, :].rearrange("p (f c) -> p f c", f=FC),
                      w_down_l[ex_ * FC:(ex_ + 1) * FC, :, cg * 512:(cg + 1) * 512].rearrange("f p c -> p f c"),
                      reads=[w_down_l], writes=[wdt])
                for t8 in range(HT):
                    tg = hf * HT + t8
                    yp = P.next_ps()
                    for fc in range(FC):
                        P.op("pe", lambda e: e.matmul(yp[:, :], lhsT=actT[:, fc * HALF + t8 * 128:fc * HALF + (t8 + 1) * 128],
                                                      rhs=wdt[:, fc * 512:(fc + 1) * 512],
                                                      start=(fc == 0), stop=(fc == FC - 1)), reads=[actT, wdt], writes=[yp])
                    ysl = yacc[:, t8 * D + cg * 512:t8 * D + (cg + 1) * 512]
                    P.op("dve", lambda e: e.scalar_tensor_tensor(out=ysl, in0=yp[:, :], scalar=G[:, tg * E + ex_:tg * E + ex_ + 1],
                                                                 in1=ysl, op0=ALU.mult, op1=ALU.add),
                         reads=[yp, G, yacc], writes=[yacc])
        P.pop()
        P.push()
        g2b = P.sb([128, D], F32, "g2b")
        P.dma("sp", g2b[:], modrow[5 * DC:6 * DC, :].rearrange("(o a) b -> o (a b)", o=1).partition_broadcast(128),
              reads=[modrow], writes=[g2b])
        fgb = P.sb([128, D], F32, "fgb")
        P.dma("sp", fgb[:], fg_row[0:1, :].partition_broadcast(128), reads=[fg_row], writes=[fgb])
        x2t = [P.sb([128, D], F32, "x2t") for _ in range(2)]
        junk3 = P.sb([128, D], BF16, "junk3")
        ss3 = P.sb([128, 1], F32, "ss3")
        rs3 = P.sb([128, 1], F32, "rs3")
        for t8 in range(HT):
            tg = hf * HT + t8
            xx = x2t[t8 % 2]
            P.dma("sp", xx[:], X2[tg * 128:(tg + 1) * 128, :], reads=[X2k[tg]], writes=[xx])
            ysl = yacc[:, t8 * D:(t8 + 1) * D]
            tt_(ysl, ysl, g2b[:], ALU.mult, [yacc, g2b], [yacc])
            tt_(xx[:], xx[:], ysl, ALU.add, [xx, yacc], [xx])
            P.op("act", lambda e: e.activation(out=junk3[:], in_=xx[:], func=AF.Square, accum_out=ss3[:, 0:1]),
                 reads=[xx], writes=[junk3, ss3])
            rstd_from_ss(ss3, ss3[:, 0:1], D, rs3, rs3[:, 0:1])
            P.op("dve", lambda e: e.scalar_tensor_tensor(out=xx[:], in0=xx[:], scalar=rs3[:, 0:1], in1=fgb[:],
                                                         op0=ALU.mult, op1=ALU.mult), reads=[xx, rs3, fgb], writes=[xx])
            P.dma("sp", out_d[tg * 128:(tg + 1) * 128, :], xx[:], reads=[xx], writes=[outk[tg]])
        P.pop()
        P.pop()
    P.close()
    return nc


def _col(v, nchunks):
    return np.ascontiguousarray(np.asarray(v, np.float32).reshape(nchunks, 128).T)


def _wchunks(w, ncol_chunks):
    K, N = w.shape
    a = np.asarray(w, np.float32).reshape(K // 128, 128, ncol_chunks, 128)
    return np.ascontiguousarray(a.transpose(2, 1, 0, 3).reshape(ncol_chunks, 128, (K // 128) * 128))


def _rows(w):
    K, N = w.shape
    a = np.asarray(w, np.float32).reshape(K // 128, 128, N)
    return np.ascontiguousarray(a.transpose(1, 0, 2).reshape(128, (K // 128) * N))


def prepare_inputs(cfg, x, c, w_ada, b_ada, norm1_g, w_in, lambda_re, lambda_im, ssm_b_re, ssm_b_im,
                   ssm_c_re, ssm_c_im, ssm_d, ssm_log_dt, w_glu, b_glu, attn_out_g, ssm_out_g,
                   w_out, norm2_g, w_router, b_router, w_gate_up, b_gate_up, w_down, b_down, final_g):
    D, TOK, NCH, NHP, WC, NT, DC, E, F, FC, INC = (cfg.D, cfg.TOK, cfg.NCH, cfg.NHP, cfg.WC, cfg.NT, cfg.DC,
                                                    cfg.E, cfg.F, cfg.FC, cfg.INC)
    f32 = np.float32
    x = np.asarray(x, f32)[0]
    L = 0
    G_ = cfg.W // 16
    common = {}
    kq = np.arange(128)
    mcur = (kq[:, None] <= kq[None, :]).astype(f32)
    mprev = (kq[:, None] >= kq[None, :]).astype(f32)
    common["mask_full"] = np.concatenate([mcur, mprev], axis=1)
    common["ident"] = np.eye(128, dtype=f32)
    common["c_col"] = _col(np.asarray(c, f32)[0], DC)
    common["w_ada_l"] = _wchunks(np.asarray(w_ada, f32)[L], 6 * DC)
    common["b_ada_col"] = _col(np.asarray(b_ada, f32)[L], 6 * DC)
    common["n1g_col"] = _col(np.asarray(norm1_g, f32)[L], DC)
    common["n2g_col"] = _col(np.asarray(norm2_g, f32)[L], DC)
    common["fg_row"] = np.asarray(final_g, f32).reshape(1, D)
    common["w_in_l"] = _wchunks(np.asarray(w_in, f32)[L], INC)

    def st_layout(a):
        a = np.asarray(a, f32).reshape(NT, 2, 64)
        return np.ascontiguousarray(a.transpose(1, 2, 0).reshape(128, NT))
    common["lam_re_l"] = st_layout(np.asarray(lambda_re, f32)[L])
    common["lam_im_l"] = st_layout(np.asarray(lambda_im, f32)[L])
    common["logdt_l"] = st_layout(np.repeat(np.asarray(ssm_log_dt, f32)[L][:, None], 64, axis=1))
    bre = np.zeros((128, NT, 128), f32)
    bim = np.zeros((128, NT, 128), f32)
    cre = np.zeros((128, NT, 128), f32)
    cim = np.zeros((128, NT, 128), f32)
    b_re = np.asarray(ssm_b_re, f32)[L]
    b_im = np.asarray(ssm_b_im, f32)[L]
    c_re = np.asarray(ssm_c_re, f32)[L]
    c_im = np.asarray(ssm_c_im, f32)[L]
    for j in range(NT):
        for gp in range(2):
            g = 2 * j + gp
            ch0 = 16 * (2 * (j % 4) + gp)
            bre[ch0:ch0 + 16, j, gp * 64:(gp + 1) * 64] = b_re[g].T
            bim[ch0:ch0 + 16, j, gp * 64:(gp + 1) * 64] = b_im[g].T
            cre[gp * 64:(gp + 1) * 64, j, ch0:ch0 + 16] = c_re[g].T
            cim[gp * 64:(gp + 1) * 64, j, ch0:ch0 + 16] = c_im[g].T
    common["bre_l"] = bre.reshape(128, NT * 128)
    common["bim_l"] = bim.reshape(128, NT * 128)
    common["cre_l"] = cre.reshape(128, NT * 128)
    common["cim_l"] = cim.reshape(128, NT * 128)
    common["dskip_col"] = _col(np.asarray(ssm_d, f32)[L].reshape(-1), WC)
    common["w_glu_l"] = _rows(np.asarray(w_glu, f32)[L])
    common["b_glu_col"] = _col(np.asarray(b_glu, f32)[L], WC)
    common["ag_col"] = _col(np.asarray(attn_out_g, f32)[L], NHP)
    common["sg_col"] = _col(np.asarray(ssm_out_g, f32)[L], WC)
    common["w_out_l"] = _rows(np.asarray(w_out, f32)[L])
    common["w_r_l"] = _rows(np.asarray(w_router, f32)[L])
    common["b_r_row"] = np.asarray(b_router, f32)[L].reshape(1, E)
    wgu = np.asarray(w_gate_up, f32)[L]
    wgl_ = np.empty((E * FC, 128, DC * 128), f32)
    wll_ = np.empty((E * FC, 128, DC * 128), f32)
    for e in range(E):
        wgl_[e * FC:(e + 1) * FC] = _wchunks(wgu[e][:, 0::2], FC)
        wll_[e * FC:(e + 1) * FC] = _wchunks(wgu[e][:, 1::2], FC)
    common["w_gate_l"] = wgl_
    common["w_lin_l"] = wll_
    bgu = np.asarray(b_gate_up, f32)[L]
    common["bg_col"] = _col(bgu[:, 0::2].reshape(-1), E * FC)
    common["bl_col"] = _col(bgu[:, 1::2].reshape(-1), E * FC)
    common["w_down_l"] = np.ascontiguousarray(np.asarray(w_down, f32)[L].reshape(E * FC, 128, D))
    common["b_down"] = np.ascontiguousarray(np.asarray(b_down, f32)[L])
    in_maps = []
    for i in range(NCORES):
        m = dict(common)
        xe = np.zeros((NCH * TOK, D), f32)
        val = np.zeros((128, NCH), f32)
        for jc in range(NCH):
            gchunk = i - (NCH - 1) + jc
            if gchunk >= 0:
                xe[jc * TOK:(jc + 1) * TOK] = x[gchunk * TOK:(gchunk + 1) * TOK]
                val[:, jc] = 1.0
        m["x_ext"] = xe
        m["valid"] = val
        mf = common["mask_full"].copy()
        if i == 0:
            mf[:, 128:256] = 0.0
        m["mask_first"] = mf
        in_maps.append(m)
    return in_maps


_NC_CACHE = {}


def run(cfg, inputs):
    key = (cfg.D, cfg.TOK, cfg.E, cfg.F)
    if key not in _NC_CACHE:
        _NC_CACHE[key] = build_program(cfg)
    nc = _NC_CACHE[key]
    in_maps = prepare_inputs(cfg, **inputs)
    res = run_bass_kernel_spmd(nc, in_maps, core_ids=list(range(NCORES)))
    global LAST_RES
    LAST_RES = res
    out = np.concatenate([np.asarray(r["out"], np.float32) for r in res.results], axis=0)
    return out.reshape(1, NCORES * cfg.TOK, cfg.D)


def kernel(**inputs):
    cfg = Cfg(D=2048, TOK=2048, E=32, F=2048)
    return run(cfg, inputs)
```

```python
import contextlib
import math
import numpy as np
import concourse.bass as bass
import concourse.mybir as mybir
from concourse.bass_utils import run_bass_kernel_spmd

F32 = mybir.dt.float32
BF16 = mybir.dt.bfloat16
I32 = mybir.dt.int32
ALU = mybir.AluOpType
AF = mybir.ActivationFunctionType
AX = mybir.AxisListType

NCORES = 8
HEAD_DIM = 64
BRANCH_DIL = (1, 4, 16)
NORM_EPS = 1e-6
SW_LIMIT = 7.0
SW_ALPHA = 1.702
TOP_K = 4


class Cfg:
    def __init__(self, D=2048, TOK=2048, E=32, F=None, debug=False):
        self.debug = debug
        self.D = D
        self.TOK = TOK
        self.NCH = NCORES
        self.A = D // 2
        self.W = D // 2
        self.NHP = self.A // 128
        self.WC = self.W // 128
        self.NT = self.W // 32
        self.DC = D // 128
        self.E = E
        self.F = F or D
        self.FC = self.F // 128
        self.INC = 3 * self.NHP + self.WC


class Buf:
    __slots__ = ("t", "last_w", "readers")

    def __init__(self, t):
        self.t = t
        self.last_w = None
        self.readers = []

    def __getitem__(self, k):
        return self.t[k]


class Prog:
    NS = 8

    def __init__(self, nc):
        self.nc = nc
        self.es = contextlib.ExitStack()
        self.stacks = [self.es]
        self.eng = dict(pe=nc.tensor, act=nc.scalar, dve=nc.vector, pool=nc.gpsimd, sp=nc.sync)
        self.csem = {e: self.es.enter_context(nc.semaphore("c_" + e)) for e in ("pe", "act", "dve", "pool")}
        self.ccnt = {e: 0 for e in self.csem}
        self.dsem = {q: [self.es.enter_context(nc.semaphore("d_%s%d" % (q, i))) for i in range(self.NS)]
                     for q in ("sp", "pool", "act")}
        self.dcnt = {q: 0 for q in self.dsem}
        self.seen = {e: {} for e in self.eng}
        self.nbuf = 0
        self.psF = []
        self.psi = 0

    def push(self):
        s = contextlib.ExitStack()
        self.stacks.append(s)

    def pop(self):
        self.barrier()
        self.stacks.pop().close()

    def sb(self, shape, dt, name="sb"):
        self.nbuf += 1
        t = self.stacks[-1].enter_context(self.nc.sbuf_tensor("%s_%d" % (name, self.nbuf), list(shape), dt))
        return Buf(t)

    def ps(self, shape, dt, name="ps"):
        self.nbuf += 1
        t = self.stacks[-1].enter_context(self.nc.psum_tensor("%s_%d" % (name, self.nbuf), list(shape), dt))
        return Buf(t)

    def dram(self, name, shape, dt, kind="Internal"):
        t = self.nc.dram_tensor(name, list(shape), dt, kind=kind)
        return Buf(t.ap())

    def next_ps(self):
        b = self.psF[self.psi % len(self.psF)]
        self.psi += 1
        return b

    def _wait(self, e, ev):
        sem, val, _ = ev
        k = id(sem)
        if self.seen[e].get(k, 0) >= val:
            return
        self.eng[e].wait_ge(sem, val)
        self.seen[e][k] = val

    def _deps(self, e, reads, writes):
        evs = []
        for b in reads:
            if b.last_w is not None:
                evs.append(b.last_w)
        for b in writes:
            if b.last_w is not None:
                evs.append(b.last_w)
            evs.extend(b.readers)
        for ev in evs:
            if e == "pe" and ev[2] == "pe":
                continue
            self._wait(e, ev)

    def _commit(self, ev, reads, writes):
        for b in writes:
            b.last_w = ev
            b.readers = []
        for b in reads:
            if b not in writes:
                b.readers.append(ev)
                if len(b.readers) > 96:
                    best = {}
                    for r in b.readers:
                        k = id(r[0])
                        if k not in best or best[k][1] < r[1]:
                            best[k] = r
                    b.readers = list(best.values())

    def op(self, e, fn, reads=(), writes=()):
        self._deps(e, reads, writes)
        ins = fn(self.eng[e])
        self.ccnt[e] += 1
        ins.then_inc(self.csem[e], 1)
        ev = (self.csem[e], self.ccnt[e], e)
        self._commit(ev, reads, writes)
        return ev

    def dma(self, q, out, in_, reads=(), writes=(), **kw):
        self._deps(q, reads, writes)
        i = self.dcnt[q]
        slot = i % self.NS
        rnd = i // self.NS
        sem = self.dsem[q][slot]
        if rnd > 0:
            self._wait(q, (sem, 16 * rnd, "dma"))
        ins = self.eng[q].dma_start(out=out, in_=in_, **kw)
        ins.then_inc(sem, 16)
        self.dcnt[q] += 1
        ev = (sem, 16 * (rnd + 1), "dma")
        self._commit(ev, reads, writes)
        return ev

    def barrier(self):
        evs = []
        for e in self.csem:
            if self.ccnt[e] > 0:
                evs.append((self.csem[e], self.ccnt[e], e))
        for q in self.dsem:
            for s in range(self.NS):
                n = (self.dcnt[q] - s + self.NS - 1) // self.NS
                if n > 0:
                    evs.append((self.dsem[q][s], 16 * n, "dma"))
        for e in self.eng:
            for ev in evs:
                self._wait(e, ev)

    def close(self):
        self.barrier()
        self.es.close()


def build_program(cfg):
    D, TOK, NCH, A, W = cfg.D, cfg.TOK, cfg.NCH, cfg.A, cfg.W
    NHP, WC, NT, DC, E, F, FC, INC = cfg.NHP, cfg.WC, cfg.NT, cfg.DC, cfg.E, cfg.F, cfg.FC, cfg.INC
    NTT = TOK // 128
    NST = TOK // 512
    TALL = NCH * TOK

    nc = bass.Bass("TRN2", target_bir_lowering=False)
    P = Prog(nc)

    def din(name, shape, dt=F32):
        return P.dram(name, shape, dt, kind="ExternalInput")

    x_ext = din("x_ext", [TALL, D])
    valid_in = din("valid", [128, NCH])
    mfirst_in = din("mask_first", [128, 256])
    mfull_in = din("mask_full", [128, 256])
    ident_in = din("ident", [128, 128])
    c_col_in = din("c_col", [128, DC])
    w_ada_l = din("w_ada_l", [6 * DC, 128, DC * 128])
    b_ada_col = din("b_ada_col", [128, 6 * DC])
    n1g_col = din("n1g_col", [128, DC])
    n1g_row = din("n1g_row", [1, D])
    n2g_col = din("n2g_col", [128, DC])
    fg_row = din("fg_row", [1, D])
    w_in_l = din("w_in_l", [INC, 128, DC * 128])
    lam_re_l = din("lam_re_l", [128, NT])
    lam_im_l = din("lam_im_l", [128, NT])
    logdt_l = din("logdt_l", [128, NT])
    bre_l = din("bre_l", [128, NT * 128])
    bim_l = din("bim_l", [128, NT * 128])
    cre_l = din("cre_l", [128, NT * 128])
    cim_l = din("cim_l", [128, NT * 128])
    dskip_col = din("dskip_col", [128, WC])
    w_glu_l = din("w_glu_l", [128, WC * W])
    b_glu_col = din("b_glu_col", [128, WC])
    ag_col = din("ag_col", [128, NHP])
    sg_col = din("sg_col", [128, WC])
    w_out_l = din("w_out_l", [128, (NHP + WC) * D])
    w_r_l = din("w_r_l", [128, DC * E])
    b_r_row = din("b_r_row", [1, E])
    w_gate_l = din("w_gate_l", [E * FC, 128, DC * 128])
    w_lin_l = din("w_lin_l", [E * FC, 128, DC * 128])
    bg_col = din("bg_col", [128, E * FC])
    bl_col = din("bl_col", [128, E * FC])
    w_down_l = din("w_down_l", [E * FC, 128, D])
    b_down = din("b_down", [E, D])
    out_d = P.dram("out", [TOK, D], F32, kind="ExternalOutput")

    SK = "ExternalOutput" if cfg.debug else "Internal"
    PT = P.dram("PT", [INC * 128, TALL], BF16, kind=SK)
    PTk = [Buf(None) for _ in range(INC)]
    X2k = [Buf(None) for _ in range(NTT)]
    outk = [Buf(None) for _ in range(NTT)]
    modrow = P.dram("modrow", [6 * DC, 128], F32, kind=SK)
    X2 = P.dram("X2", [TOK, D], F32, kind=SK)
    H2T = P.dram("H2T", [DC * 128, TOK], BF16, kind=SK)
    DBG = P.dram("DBG", [128, 8 * TOK], F32, kind=SK)

    P.psF = [P.ps([128, 512], F32, "psF") for _ in range(6)]
    psB = [P.ps([128, 1024], BF16, "psB") for _ in range(2)]

    ident_f = P.sb([128, 128], F32, "identf")
    ident_b = P.sb([128, 128], BF16, "identb")
    ones_f = P.sb([128, 128], F32, "onesf")
    ones_b = P.sb([128, 128], BF16, "onesb")
    valid = P.sb([128, NCH], F32, "valid")
    modT = P.sb([128, 6 * DC], F32, "modT")
    s1c = P.sb([128, DC], F32, "s1c")
    s2c = P.sb([128, DC], F32, "s2c")
    G = P.sb([128, NTT * E], F32, "gates")
    rstd_a = P.sb([128, NTT], F32, "rstda")
    rstd_s = P.sb([128, NTT], F32, "rstds")

    P.dma("sp", ident_f[:], ident_in[:, :], reads=[ident_in], writes=[ident_f])
    P.dma("sp", valid[:], valid_in[:, :], reads=[valid_in], writes=[valid])
    P.op("dve", lambda e: e.tensor_copy(out=ident_b[:], in_=ident_f[:]), reads=[ident_f], writes=[ident_b])
    P.op("dve", lambda e: e.memset(ones_f[:], 1.0), writes=[ones_f])
    P.op("dve", lambda e: e.memset(ones_b[:], 1.0), writes=[ones_b])

    def rstd_from_ss(ssb, ss, n, outb, out, width=1):
        t = P.sb([128, width], F32, "rs_t")
        P.op("dve", lambda e: e.tensor_scalar(out=t[:], in0=ss, scalar1=1.0 / n, scalar2=NORM_EPS,
                                               op0=ALU.mult, op1=ALU.add), reads=[ssb], writes=[t])
        P.op("act", lambda e: e.activation(out=t[:], in_=t[:], func=AF.Sqrt), reads=[t], writes=[t])
        P.op("dve", lambda e: e.reciprocal(out=out, in_=t[:]), reads=[t], writes=[outb])

    P.push()
    c_act = P.sb([128, DC], F32, "cact")
    b_ada = P.sb([128, 6 * DC], F32, "bada")
    n1g = P.sb([128, DC], F32, "n1g")
    n2g = P.sb([128, DC], F32, "n2g")
    P.dma("sp", c_act[:], c_col_in[:, :], reads=[c_col_in], writes=[c_act])
    P.dma("sp", b_ada[:], b_ada_col[:, :], reads=[b_ada_col], writes=[b_ada])
    P.dma("sp", n1g[:], n1g_col[:, :], reads=[n1g_col], writes=[n1g])
    P.dma("sp", n2g[:], n2g_col[:, :], reads=[n2g_col], writes=[n2g])
    P.op("act", lambda e: e.activation(out=c_act[:], in_=c_act[:], func=AF.Silu), reads=[c_act], writes=[c_act])
    wa = [P.sb([128, DC * 128], F32, "wa") for _ in range(2)]
    mps = P.next_ps()
    for oc in range(6 * DC):
        wt = wa[oc % 2]
        P.dma("sp", wt[:], w_ada_l[oc, :, :], reads=[w_ada_l], writes=[wt])
        for k in range(DC):
            P.op("pe", lambda e: e.matmul(mps[:, oc:oc + 1], lhsT=wt[:, k * 128:(k + 1) * 128],
                                          rhs=c_act[:, k:k + 1], start=(k == 0), stop=(k == DC - 1)),
                 reads=[wt, c_act], writes=[mps])
    P.op("dve", lambda e: e.tensor_tensor(out=modT[:], in0=mps[:, 0:6 * DC], in1=b_ada[:], op=ALU.add),
         reads=[mps, b_ada], writes=[modT])
    P.op("dve", lambda e: e.scalar_tensor_tensor(out=s1c[:], in0=modT[:, DC:2 * DC], scalar=1.0, in1=n1g[:],
                                                 op0=ALU.add, op1=ALU.mult), reads=[modT, n1g], writes=[s1c])
    P.op("dve", lambda e: e.scalar_tensor_tensor(out=s2c[:], in0=modT[:, 4 * DC:5 * DC], scalar=1.0, in1=n2g[:],
                                                 op0=ALU.add, op1=ALU.mult), reads=[modT, n2g], writes=[s2c])
    tps = P.next_ps()
    P.op("pe", lambda e: e.transpose(tps[0:6 * DC, 0:128], modT[:, :], ident_f[:]), reads=[modT, ident_f], writes=[tps])
    mrow = P.sb([128, 128], F32, "mrow")
    P.op("dve", lambda e: e.tensor_copy(out=mrow[0:6 * DC, :], in_=tps[0:6 * DC, 0:128]), reads=[tps], writes=[mrow])
    P.dma("sp", modrow[:, :], mrow[0:6 * DC, :], reads=[mrow], writes=[modrow])
    P.pop()

    P.push()
    wu = P.sb([128, WC * DC * 128], BF16, "wu")
    for j in range(WC):
        P.dma("pool", wu[:, j * DC * 128:(j + 1) * DC * 128], w_in_l[3 * NHP + j, :, :], reads=[w_in_l], writes=[wu])
    xt = [P.sb([128, D], F32, "xt") for _ in range(4)]
    xn = [P.sb([128, D], BF16, "xn") for _ in range(4)]
    hT = [P.sb([128, DC * 512], BF16, "hT") for _ in range(2)]
    wq = [P.sb([128, DC * 128], BF16, "wq") for _ in range(3)]
    stg = [P.sb([128, 512], BF16, "stg") for _ in range(4)]
    ssqs = [P.sb([128, 1], F32, "ssq") for _ in range(8)]
    rsds = [P.sb([128, 1], F32, "rsd") for _ in range(8)]
    junks = [P.sb([128, D], BF16, "junk") for _ in range(2)]
    cnt = dict(tile=0, w=0, st=0, stg=0, pb=0)
    s1row = P.sb([128, D], F32, "s1row")
    sh1row = P.sb([128, D], F32, "sh1row")
    n1row = P.sb([128, D], F32, "n1row")
    P.dma("sp", sh1row[:], modrow[0:DC, :].rearrange("(o a) b -> o (a b)", o=1).partition_broadcast(128),
          reads=[modrow], writes=[sh1row])
    P.dma("sp", s1row[:], modrow[DC:2 * DC, :].rearrange("(o a) b -> o (a b)", o=1).partition_broadcast(128),
          reads=[modrow], writes=[s1row])
    P.dma("sp", n1row[:], n1g_row[0:1, :].partition_broadcast(128), reads=[n1g_row], writes=[n1row])
    P.op("dve", lambda e: e.scalar_tensor_tensor(out=s1row[:], in0=s1row[:], scalar=1.0, in1=n1row[:],
                                                 op0=ALU.add, op1=ALU.mult), reads=[s1row, n1row], writes=[s1row])
    xm = [P.sb([128, D], F32, "xm") for _ in range(2)]
    for jc in range(NCH):
        if jc < NCH - 2:
            ccs = list(range(3 * NHP, INC))
        elif jc == NCH - 2:
            ccs = list(range(NHP, INC))
        else:
            ccs = list(range(INC))
        for st in range(NST):
            h = hT[cnt["st"] % 2]
            cnt["st"] += 1
            for t4 in range(4):
                i = t4
                cnt["tile"] += 1
                row0 = jc * TOK + st * 512 + t4 * 128
                P.dma("sp", xt[i][:], x_ext[row0:row0 + 128, :], reads=[x_ext], writes=[xt[i]])
                ssq = ssqs[cnt["tile"] % 8]
                rsd = rsds[cnt["tile"] % 8]
                junk = junks[t4 % 2]
                P.op("act", lambda e: e.activation(out=junk[:], in_=xt[i][:], func=AF.Square,
                                                   accum_out=ssq[:, 0:1]), reads=[xt[i]], writes=[junk, ssq])
                rstd_from_ss(ssq, ssq[:, 0:1], D, rsd, rsd[:, 0:1])
                xm_ = xm[t4 % 2]
                P.op("dve", lambda e: e.scalar_tensor_tensor(out=xm_[:], in0=xt[i][:], scalar=rsd[:, 0:1], in1=s1row[:],
                                                             op0=ALU.mult, op1=ALU.mult), reads=[xt[i], rsd, s1row], writes=[xm_])
                P.op("dve", lambda e: e.tensor_tensor(out=xn[i][:], in0=xm_[:], in1=sh1row[:], op=ALU.add),
                     reads=[xm_, sh1row], writes=[xn[i]])
            for t4 in range(4):
                i = t4
                for kb in range(0, DC, 8):
                    nk = min(8, DC - kb)
                    pb = psB[cnt["pb"] % 2]
                    cnt["pb"] += 1
                    for kk in range(nk):
                        k = kb + kk
                        P.op("pe", lambda e: e.transpose(pb[:, kk * 128:(kk + 1) * 128], xn[i][:, k * 128:(k + 1) * 128],
                                                         ident_b[:]), reads=[xn[i], ident_b], writes=[pb])
                    hv_ = h[:, :].rearrange("p (k t) -> p k t", k=DC)[:, kb:kb + nk, t4 * 128:(t4 + 1) * 128]
                    pv_ = pb[:, 0:nk * 128].rearrange("p (k t) -> p k t", k=nk)
                    if cnt["pb"] % 2 == 0:
                        P.op("dve", lambda e: e.tensor_copy(out=hv_, in_=pv_), reads=[pb], writes=[h])
                    else:
                        P.op("act", lambda e: e.activation(out=hv_, in_=pv_, func=AF.Identity), reads=[pb], writes=[h])
            col0 = jc * TOK + st * 512
            for cc in ccs:
                if cc >= 3 * NHP:
                    j = cc - 3 * NHP
                    wt, wofs = wu, j * DC * 128
                else:
                    wt = wq[cnt["w"] % 3]
                    cnt["w"] += 1
                    wofs = 0
                    P.dma("pool", wt[:], w_in_l[cc, :, :], reads=[w_in_l], writes=[wt])
                pp = P.next_ps()
                for k in range(DC):
                    P.op("pe", lambda e: e.matmul(pp[:, :], lhsT=wt[:, wofs + k * 128:wofs + (k + 1) * 128],
                                                  rhs=h[:, k * 512:(k + 1) * 512], start=(k == 0), stop=(k == DC - 1)),
                         reads=[wt, h], writes=[pp])
                sg_ = stg[cnt["stg"] % 4]
                cnt["stg"] += 1
                if cc >= 3 * NHP:
                    P.op("act", lambda e: e.activation(out=sg_[:], in_=pp[:, :], func=AF.Identity,
                                                       scale=valid[:, jc:jc + 1]), reads=[pp, valid], writes=[sg_])
                else:
                    P.op("act", lambda e: e.activation(out=sg_[:], in_=pp[:, :], func=AF.Identity),
                         reads=[pp], writes=[sg_])
                P.dma("act", PT[cc * 128:(cc + 1) * 128, col0:col0 + 512], sg_[:], reads=[sg_], writes=[PTk[cc]])
    P.pop()

    P.push()
    ymixT = P.sb([128, (NHP + WC) * TOK], BF16, "ymixT")

    P.push()
    HO = (NCH - 2) * TOK
    mfull = P.sb([128, 256], BF16, "mfull")
    mfirst = P.sb([128, 256], BF16, "mfirst")
    P.dma("pool", mfull[:], mfull_in[:, :], reads=[mfull_in], writes=[mfull])
    P.dma("pool", mfirst[:], mfirst_in[:, :], reads=[mfirst_in], writes=[mfirst])
    agc = P.sb([128, NHP], F32, "agc")
    P.dma("sp", agc[:], ag_col[:, :], reads=[ag_col], writes=[agc])
    kb_index = {}
    for b, d in enumerate(BRANCH_DIL):
        span = 128 * d
        for n in range(-1, TOK // span):
            for r in range(d):
                kb_index[(b, n, r)] = len(kb_index)
    NKB = len(kb_index)
    QT = P.sb([128, TOK], BF16, "QT")
    KT = P.sb([128, 2 * TOK], BF16, "KT")
    VT = P.sb([128, 2 * TOK], BF16, "VT")
    VB = P.sb([128, NKB * 128], BF16, "VB")
    acc = P.sb([128, 2 * TOK], F32, "acc")
    ptile = [P.sb([128, 256], BF16, "ptile") for _ in range(4)]
    ptm = [P.sb([128, 256], BF16, "ptm") for _ in range(4)]
    ysq = P.sb([128, TOK], F32, "ysq")
    ssa = P.sb([128, NTT], F32, "ssa")
    sst = P.sb([128, NTT], F32, "sst")
    pcnt = 0
    blk_cnt = 0
    psH = []
    for b_ in P.psF:
        psH.append(Buf(b_.t[:, 0:256]))
        psH.append(Buf(b_.t[:, 256:512]))
    for hp in range(NHP):
        P.dma("sp", QT[:], PT[hp * 128:(hp + 1) * 128, HO + TOK:HO + 2 * TOK], reads=[PTk[hp]], writes=[QT])
        P.dma("sp", KT[:], PT[(NHP + hp) * 128:(NHP + hp + 1) * 128, HO:HO + 2 * TOK], reads=[PTk[NHP + hp]], writes=[KT])
        P.dma("sp", VT[:], PT[(2 * NHP + hp) * 128:(2 * NHP + hp + 1) * 128, HO:HO + 2 * TOK], reads=[PTk[2 * NHP + hp]], writes=[VT])

        def kpos(b, n, r):
            d = BRANCH_DIL[b]
            base = TOK + n * 128 * d + r
            return base, d

        items = list(kb_index.items())
        for g0 in range(0, NKB, 8):
            pb = psB[(g0 // 8) % 2]
            grp = items[g0:g0 + 8]
            for ii, ((b, n, r), idx) in enumerate(grp):
                base, d = kpos(b, n, r)
                P.op("pe", lambda e: e.transpose(pb[:, ii * 128:(ii + 1) * 128],
                                                 VT[:, base:base + 127 * d + 1:d], ident_b[:]),
                     reads=[VT, ident_b], writes=[pb])
            ng = len(grp)
            P.op("act", lambda e: e.activation(out=VB[:, g0 * 128:(g0 + ng) * 128], in_=pb[:, 0:ng * 128],
                                               func=AF.Identity), reads=[pb], writes=[VB])
        for b, d in enumerate(BRANCH_DIL):
            span = 128 * d
            for n in range(TOK // span):
                for r in range(d):
                    cb, _ = kpos(b, n, r)
                    pv, _ = kpos(b, n - 1, r)
                    qb = n * span + r
                    ci = kb_index[(b, n, r)]
                    pi = kb_index[(b, n - 1, r)]
                    hb_ = blk_cnt % 4
                    O = psH[8 + blk_cnt % 3]
                    blk_cnt += 1
                    for a in range(2):
                        lo, hi = a * 64, (a + 1) * 64
                        S = psH[4 * a + hb_]
                        P.op("pe", lambda e: e.matmul(S[:, 0:128], lhsT=KT[lo:hi, cb:cb + 127 * d + 1:d],
                                                      rhs=QT[lo:hi, qb:qb + 127 * d + 1:d], start=True, stop=True),
                             reads=[KT, QT], writes=[S])
                        P.op("pe", lambda e: e.matmul(S[:, 128:256], lhsT=KT[lo:hi, pv:pv + 127 * d + 1:d],
                                                      rhs=QT[lo:hi, qb:qb + 127 * d + 1:d], start=True, stop=True),
                             reads=[KT, QT], writes=[S])
                        pt = ptile[pcnt % 4]
                        pm = ptm[pcnt % 4]
                        pcnt += 1
                        P.op("act", lambda e: e.activation(out=pt[:], in_=S[:, 0:256], func=AF.Exp,
                                                           scale=HEAD_DIM ** -0.5), reads=[S], writes=[pt])
                        mk = mfirst if n == 0 else mfull
                        P.op("dve", lambda e: e.tensor_tensor(out=pm[:], in0=pt[:], in1=mk[:], op=ALU.mult),
                             reads=[pt, mk], writes=[pm])
                        P.op("pe", lambda e: e.matmul(O[lo:hi, 0:128], lhsT=VB[:, ci * 128 + lo:ci * 128 + hi],
                                                      rhs=pm[:, 0:128], start=True, stop=False), reads=[VB, pm], writes=[O])
                        P.op("pe", lambda e: e.matmul(O[lo:hi, 0:128], lhsT=VB[:, pi * 128 + lo:pi * 128 + hi],
                                                      rhs=pm[:, 128:256], start=False, stop=True), reads=[VB, pm], writes=[O])
                        P.op("pe", lambda e: e.matmul(O[lo:hi, 128:256], lhsT=ones_b[:, 0:64],
                                                      rhs=pm[:, 0:128], start=True, stop=False), reads=[ones_b, pm], writes=[O])
                        P.op("pe", lambda e: e.matmul(O[lo:hi, 128:256], lhsT=ones_b[:, 0:64],
                                                      rhs=pm[:, 128:256], start=False, stop=True), reads=[ones_b, pm], writes=[O])
                    av = acc[:, :].rearrange("p (c t) -> p c t", c=2)[:, :, qb:qb + 127 * d + 1:d]
                    ov = O[:, :].rearrange("p (c t) -> p c t", c=2)
                    if b == 0:
                        P.op("dve", lambda e: e.tensor_copy(out=av, in_=ov), reads=[O], writes=[acc])
                    else:
                        P.op("dve", lambda e: e.tensor_tensor(out=av, in0=ov, in1=av, op=ALU.add),
                             reads=[O, acc], writes=[acc])
        P.op("dve", lambda e: e.reciprocal(out=acc[:, TOK:2 * TOK], in_=acc[:, TOK:2 * TOK]), reads=[acc], writes=[acc])
        P.op("dve", lambda e: e.tensor_tensor(out=acc[:, 0:TOK], in0=acc[:, 0:TOK], in1=acc[:, TOK:2 * TOK], op=ALU.mult),
             reads=[acc], writes=[acc])
        if cfg.debug and hp < 4:
            P.dma("sp", DBG[:, hp * TOK:(hp + 1) * TOK], acc[:, 0:TOK], reads=[acc], writes=[DBG])
        P.op("pool", lambda e: e.tensor_tensor(out=ysq[:], in0=acc[:, 0:TOK], in1=acc[:, 0:TOK], op=ALU.mult),
             reads=[acc], writes=[ysq])
        sp_ = psH[11]
        for tt in range(NTT):
            P.op("pe", lambda e: e.matmul(sp_[:, tt:tt + 1], lhsT=ysq[:, tt * 128:(tt + 1) * 128], rhs=ones_f[:, 0:1],
                                          start=True, stop=True), reads=[ysq, ones_f], writes=[sp_])
        if hp == 0:
            P.op("dve", lambda e: e.tensor_copy(out=ssa[:], in_=sp_[:, 0:NTT]), reads=[sp_], writes=[ssa])
        else:
            P.op("dve", lambda e: e.tensor_tensor(out=ssa[:], in0=sp_[:, 0:NTT], in1=ssa[:], op=ALU.add),
                 reads=[sp_, ssa], writes=[ssa])
        P.op("act", lambda e: e.activation(out=ymixT[:, hp * TOK:(hp + 1) * TOK], in_=acc[:, 0:TOK], func=AF.Identity,
                                           scale=agc[:, hp:hp + 1]), reads=[acc, agc], writes=[ymixT])
    rstd_from_ss(ssa, ssa[:], A, rstd_a, rstd_a[:], width=NTT)
    P.pop()

    P.push()
    lre = P.sb([128, NT], F32, "lre")
    lim = P.sb([128, NT], F32, "lim")
    dtt = P.sb([128, NT], F32, "dtt")
    P.dma("sp", lre[:], lam_re_l[:, :], reads=[lam_re_l], writes=[lre])
    P.dma("sp", lim[:], lam_im_l[:, :], reads=[lam_im_l], writes=[lim])
    P.dma("sp", dtt[:], logdt_l[:, :], reads=[logdt_l], writes=[dtt])
    rho = P.sb([128, NT], F32, "rho")
    th = P.sb([128, NT], F32, "th")
    t0 = P.sb([128, NT], F32, "t0")
    t1 = P.sb([128, NT], F32, "t1")
    ti = P.sb([128, NT], I32, "ti")
    ck = P.sb([128, 12 * NT], F32, "ck")
    sk = P.sb([128, 12 * NT], F32, "sk")
    fre = P.sb([128, NT], F32, "fre")
    fim = P.sb([128, NT], F32, "fim")

    def tt_(out, a, b, op, rd, wr, eng="dve"):
        P.op(eng, lambda e: e.tensor_tensor(out=out, in0=a, in1=b, op=op), reads=rd, writes=wr)

    def ts_(out, a, s1, s2, op0, op1, rd, wr, eng="dve"):
        if s2 is None:
            P.op(eng, lambda e: e.tensor_scalar(out=out, in0=a, scalar1=s1, scalar2=None, op0=op0), reads=rd, writes=wr)
        else:
            P.op(eng, lambda e: e.tensor_scalar(out=out, in0=a, scalar1=s1, scalar2=s2, op0=op0, op1=op1), reads=rd, writes=wr)

    P.op("act", lambda e: e.activation(out=dtt[:], in_=dtt[:], func=AF.Exp), reads=[dtt], writes=[dtt])
    tt_(rho[:], lre[:], dtt[:], ALU.mult, [lre, dtt], [rho])
    P.op("act", lambda e: e.activation(out=rho[:], in_=rho[:], func=AF.Exp), reads=[rho], writes=[rho])
    tt_(th[:], lim[:], dtt[:], ALU.mult, [lim, dtt], [th])

    t2 = P.sb([128, NT], F32, "t2")

    def frac_pm_half(dstb, srcb):
        P.op("dve", lambda e: e.tensor_copy(out=ti[:], in_=srcb[:]), reads=[srcb], writes=[ti])
        P.op("dve", lambda e: e.tensor_copy(out=dstb[:], in_=ti[:]), reads=[ti], writes=[dstb])
        tt_(dstb[:], srcb[:], dstb[:], ALU.subtract, [srcb, dstb], [dstb])
        P.op("dve", lambda e: e.scalar_tensor_tensor(out=srcb[:], in0=dstb[:], scalar=0.5, in1=dstb[:], op0=ALU.is_gt,
                                                     op1=ALU.subtract), reads=[dstb], writes=[srcb])
        ts_(dstb[:], srcb[:], -1.0, None, ALU.mult, None, [srcb], [dstb])
        P.op("dve", lambda e: e.scalar_tensor_tensor(out=srcb[:], in0=dstb[:], scalar=-0.5, in1=dstb[:], op0=ALU.is_lt,
                                                     op1=ALU.add), reads=[dstb], writes=[srcb])
        P.op("dve", lambda e: e.tensor_copy(out=dstb[:], in_=srcb[:]), reads=[srcb], writes=[dstb])

    ts_(t0[:], th[:], 1.0 / (2 * math.pi), None, ALU.mult, None, [th], [t0])
    frac_pm_half(t2, t0)
    P.op("act", lambda e: e.activation(out=sk[:, 0:NT], in_=t2[:], func=AF.Sin, scale=2 * math.pi), reads=[t2], writes=[sk])
    ts_(t1[:], th[:], 1.0 / (2 * math.pi), 0.25, ALU.mult, ALU.add, [th], [t1])
    frac_pm_half(t2, t1)
    P.op("act", lambda e: e.activation(out=ck[:, 0:NT], in_=t2[:], func=AF.Sin, scale=2 * math.pi), reads=[t2], writes=[ck])
    for k in range(11):
        c0, s0 = ck[:, k * NT:(k + 1) * NT], sk[:, k * NT:(k + 1) * NT]
        c1, s1_ = ck[:, (k + 1) * NT:(k + 2) * NT], sk[:, (k + 1) * NT:(k + 2) * NT]
        tt_(t0[:], c0, c0, ALU.mult, [ck], [t0])
        tt_(t1[:], s0, s0, ALU.mult, [sk], [t1])
        tt_(c1, t0[:], t1[:], ALU.subtract, [t0, t1], [ck])
        tt_(t0[:], c0, s0, ALU.mult, [ck, sk], [t0])
        ts_(s1_, t0[:], 2.0, None, ALU.mult, None, [t0], [sk])
    abr = P.sb([128, NT], F32, "abr")
    abi = P.sb([128, NT], F32, "abi")
    den = P.sb([128, NT], F32, "den")
    tt_(abr[:], rho[:], ck[:, 0:NT], ALU.mult, [rho, ck], [abr])
    ts_(abr[:], abr[:], -1.0, None, ALU.add, None, [abr], [abr])
    tt_(abi[:], rho[:], sk[:, 0:NT], ALU.mult, [rho, sk], [abi])
    tt_(t0[:], lre[:], lre[:], ALU.mult, [lre], [t0])
    tt_(t1[:], lim[:], lim[:], ALU.mult, [lim], [t1])
    tt_(den[:], t0[:], t1[:], ALU.add, [t0, t1], [den])
    P.op("dve", lambda e: e.reciprocal(out=den[:], in_=den[:]), reads=[den], writes=[den])
    tt_(t0[:], abr[:], lre[:], ALU.mult, [abr, lre], [t0])
    tt_(t1[:], abi[:], lim[:], ALU.mult, [abi, lim], [t1])
    tt_(t0[:], t0[:], t1[:], ALU.add, [t0, t1], [t0])
    tt_(fre[:], t0[:], den[:], ALU.mult, [t0, den], [fre])
    tt_(t0[:], abi[:], lre[:], ALU.mult, [abi, lre], [t0])
    tt_(t1[:], abr[:], lim[:], ALU.mult, [abr, lim], [t1])
    tt_(t0[:], t0[:], t1[:], ALU.subtract, [t0, t1], [t0])
    tt_(fim[:], t0[:], den[:], ALU.mult, [t0, den], [fim])

    Bre = P.sb([128, NT * 128], BF16, "Bre")
    Bim = P.sb([128, NT * 128], BF16, "Bim")
    P.dma("pool", Bre[:], bre_l[:, :], reads=[bre_l], writes=[Bre], max_dma_last_dim=4096)
    P.dma("pool", Bim[:], bim_l[:, :], reads=[bim_l], writes=[Bim], max_dma_last_dim=4096)
    Cr = P.sb([128, NT * 128], BF16, "Cr")
    Ci = P.sb([128, NT * 128], BF16, "Ci")
    P.push()
    Craw_r = P.sb([128, NT * 128], F32, "Crr")
    Craw_i = P.sb([128, NT * 128], F32, "Cri")
    P.dma("sp", Craw_r[:], cre_l[:, :], reads=[cre_l], writes=[Craw_r])
    P.dma("sp", Craw_i[:], cim_l[:, :], reads=[cim_l], writes=[Craw_i])
    ctmp = P.sb([128, 128], F32, "ctmp")
    for j in range(NT):
        sl = slice(j * 128, (j + 1) * 128)
        ts_(ctmp[:], Craw_i[:, sl], fim[:, j:j + 1], None, ALU.mult, None, [Craw_i, fim], [ctmp])
        P.op("dve", lambda e: e.scalar_tensor_tensor(out=Cr[:, sl], in0=Craw_r[:, sl], scalar=fre[:, j:j + 1], in1=ctmp[:],
                                                     op0=ALU.mult, op1=ALU.subtract), reads=[Craw_r, fre, ctmp], writes=[Cr])
        ts_(ctmp[:], Craw_i[:, sl], fre[:, j:j + 1], None, ALU.mult, None, [Craw_i, fre], [ctmp])
        P.op("dve", lambda e: e.scalar_tensor_tensor(out=Ci[:, sl], in0=Craw_r[:, sl], scalar=fim[:, j:j + 1], in1=ctmp[:],
                                                     op0=ALU.mult, op1=ALU.add), reads=[Craw_r, fim, ctmp], writes=[Ci])
        ts_(Ci[:, sl], Ci[:, sl], -1.0, None, ALU.mult, None, [Ci], [Ci])
    P.pop()
    dsk = P.sb([128, WC], F32, "dsk")
    P.dma("sp", dsk[:], dskip_col[:, :], reads=[dskip_col], writes=[dsk])

    kbig = int(round(math.log2(TOK)))
    NTH = TOK // 128
    rk = P.sb([128, 12 * NT], F32, "rk")
    ark = P.sb([128, 12 * NT], F32, "ark")
    aik = P.sb([128, 12 * NT], F32, "aik")
    P.op("dve", lambda e: e.tensor_copy(out=rk[:, 0:NT], in_=rho[:]), reads=[rho], writes=[rk])
    for k in range(11):
        tt_(rk[:, (k + 1) * NT:(k + 2) * NT], rk[:, k * NT:(k + 1) * NT], rk[:, k * NT:(k + 1) * NT], ALU.mult, [rk], [rk])
    tt_(ark[:], rk[:], ck[:], ALU.mult, [rk, ck], [ark])
    tt_(aik[:], rk[:], sk[:], ALU.mult, [rk, sk], [aik])
    Vst = P.sb([128, 2 * NT], F32, "Vst")
    P.op("dve", lambda e: e.memset(Vst[:], 0.0), writes=[Vst])
    P.push()
    Pr = P.sb([128, TOK], F32, "Pr")
    Pi = P.sb([128, TOK], F32, "Pi")
    ptmp = P.sb([128, TOK // 2], F32, "ptmp")
    PTt = [P.sb([128, 2 * TOK], BF16, "PTt") for _ in range(4)]
    B2 = [P.sb([128, 256], F32, "B2") for _ in range(4)]
    UTp = [P.sb([128, TOK], BF16, "UTp") for _ in range(2)]
    UTT = [P.sb([128, TOK], BF16, "UTT") for _ in range(2)]
    sacc = P.sb([128, 4], F32, "sacc")
    hv = P.sb([128, 6], F32, "hv")
    junkS = P.sb([128, 128], F32, "junkS")
    pcn = 0
    for chq in range(WC):
        for jj in range(4):
            j = chq * 4 + jj
            P.op("dve", lambda e: e.memset(Pr[:, TOK - 1:TOK], 1.0), writes=[Pr])
            P.op("dve", lambda e: e.memset(Pi[:, TOK - 1:TOK], 0.0), writes=[Pi])
            k = 0
            w = 1
            while w < TOK:
                s_ = slice(TOK - w, TOK)
                d_ = slice(TOK - 2 * w, TOK - w)
                aR = ark[:, k * NT + j:k * NT + j + 1]
                aI = aik[:, k * NT + j:k * NT + j + 1]
                ts_(ptmp[:, 0:w], Pi[:, s_], aI, None, ALU.mult, None, [Pi, aik], [ptmp])
                P.op("dve", lambda e: e.scalar_tensor_tensor(out=Pr[:, d_], in0=Pr[:, s_], scalar=aR, in1=ptmp[:, 0:w],
                                                             op0=ALU.mult, op1=ALU.subtract), reads=[Pr, ark, ptmp], writes=[Pr])
                ts_(ptmp[:, 0:w], Pr[:, s_], aI, None, ALU.mult, None, [Pr, aik], [ptmp])
                P.op("dve", lambda e: e.scalar_tensor_tensor(out=Pi[:, d_], in0=Pi[:, s_], scalar=aR, in1=ptmp[:, 0:w],
                                                             op0=ALU.mult, op1=ALU.add), reads=[Pi, aik, ptmp], writes=[Pi])
                w *= 2
                k += 1
            for hh, Psrc in enumerate((Pr, Pi)):
                for t0_ in range(0, NTH, 4):
                    tp = P.next_ps()
                    for q4 in range(4):
                        th_ = t0_ + q4
                        P.op("pe", lambda e: e.transpose(tp[:, q4 * 128:(q4 + 1) * 128], Psrc[:, th_ * 128:(th_ + 1) * 128], ident_f[:]),
                             reads=[Psrc, ident_f], writes=[tp])
                    P.op("act", lambda e: e.activation(out=PTt[jj][:, hh * TOK + t0_ * 128:hh * TOK + (t0_ + 4) * 128], in_=tp[:, :],
                                                       func=AF.Identity), reads=[tp], writes=[PTt[jj]])
            tb = psB[pcn % 2]
            pcn += 1
            P.op("pe", lambda e: e.transpose(tb[:, 0:128], Bre[:, j * 128:(j + 1) * 128], ident_b[:]), reads=[Bre, ident_b], writes=[tb])
            P.op("pe", lambda e: e.transpose(tb[:, 128:256], Bim[:, j * 128:(j + 1) * 128], ident_b[:]), reads=[Bim, ident_b], writes=[tb])
            P.op("act", lambda e: e.activation(out=B2[jj][:], in_=tb[:, 0:256], func=AF.Identity), reads=[tb], writes=[B2[jj]])
        for jc in range(NCH - 1):
            u = UTp[jc % 2]
            utt = UTT[jc % 2]
            urow = (3 * NHP + chq) * 128
            P.dma("sp", u[:], PT[urow:urow + 128, jc * TOK:(jc + 1) * TOK], reads=[PTk[3 * NHP + chq]], writes=[u])
            for g0 in range(0, NTH, 8):
                tb = psB[pcn % 2]
                pcn += 1
                for q8 in range(8):
                    th_ = g0 + q8
                    P.op("pe", lambda e: e.transpose(tb[:, q8 * 128:(q8 + 1) * 128], u[:, th_ * 128:(th_ + 1) * 128], ident_b[:]),
                         reads=[u, ident_b], writes=[tb])
                P.op("act", lambda e: e.activation(out=utt[:, g0 * 128:(g0 + 8) * 128], in_=tb[:, :], func=AF.Identity),
                     reads=[tb], writes=[utt])
            for jj in range(4):
                j = chq * 4 + jj
                mp = P.next_ps()
                for hh in range(2):
                    for th_ in range(NTH):
                        P.op("pe", lambda e: e.matmul(mp[:, hh * 128:(hh + 1) * 128],
                                                      lhsT=PTt[jj][:, hh * TOK + th_ * 128:hh * TOK + (th_ + 1) * 128],
                                                      rhs=utt[:, th_ * 128:(th_ + 1) * 128], start=(th_ == 0), stop=(th_ == NTH - 1)),
                             reads=[PTt[jj], utt], writes=[mp])
                combos = ((0, 0), (1, 1), (1, 0), (0, 1))
                for ci_, (mh, bh) in enumerate(combos):
                    P.op("dve", lambda e: e.scalar_tensor_tensor(out=junkS[:], in0=mp[:, mh * 128:(mh + 1) * 128], scalar=1.0,
                                                                 in1=B2[jj][:, bh * 128:(bh + 1) * 128], op0=ALU.mult, op1=ALU.mult,
                                                                 accum_out=sacc[:, ci_:ci_ + 1]), reads=[mp, B2[jj]], writes=[junkS, sacc])
                aR = ark[:, kbig * NT + j:kbig * NT + j + 1]
                aI = aik[:, kbig * NT + j:kbig * NT + j + 1]
                Vr = Vst[:, j:j + 1]
                Vi = Vst[:, NT + j:NT + j + 1]
                tt_(hv[:, 4:5], sacc[:, 0:1], sacc[:, 1:2], ALU.subtract, [sacc], [hv])
                tt_(hv[:, 5:6], sacc[:, 2:3], sacc[:, 3:4], ALU.add, [sacc], [hv])
                ts_(hv[:, 0:1], Vi, aI, None, ALU.mult, None, [Vst, aik], [hv])
                P.op("dve", lambda e: e.scalar_tensor_tensor(out=hv[:, 1:2], in0=Vr, scalar=aR, in1=hv[:, 0:1],
                                                             op0=ALU.mult, op1=ALU.subtract), reads=[Vst, ark, hv], writes=[hv])
                ts_(hv[:, 2:3], Vr, aI, None, ALU.mult, None, [Vst, aik], [hv])
                P.op("dve", lambda e: e.scalar_tensor_tensor(out=hv[:, 3:4], in0=Vi, scalar=aR, in1=hv[:, 2:3],
                                                             op0=ALU.mult, op1=ALU.add), reads=[Vst, ark, hv], writes=[hv])
                tt_(Vr, hv[:, 1:2], hv[:, 4:5], ALU.add, [hv], [Vst])
                tt_(Vi, hv[:, 3:4], hv[:, 5:6], ALU.add, [hv], [Vst])
    P.pop()

    P.push()
    ct = P.sb([128, TOK], F32, "ct")
    st_ = P.sb([128, TOK], F32, "st")
    UT = [P.sb([128, TOK], BF16, "UT") for _ in range(2)]
    brs = [P.sb([128, 512], F32, "brs") for _ in range(2)]
    bis = [P.sb([128, 512], F32, "bis") for _ in range(2)]
    wr_ = P.sb([128, TOK], F32, "wr")
    wi_ = P.sb([128, TOK], F32, "wi")
    tmpA = wr_
    gq = wi_
    zr = P.sb([128, TOK], F32, "zr")
    zi = P.sb([128, TOK], F32, "zi")
    pa = [P.sb([128, 512], F32, "pa") for _ in range(2)]
    pb_ = [P.sb([128, 512], F32, "pbb") for _ in range(2)]
    da = [P.sb([128, 512], F32, "da") for _ in range(2)]
    db = [P.sb([128, 512], F32, "db") for _ in range(2)]
    xrb = [P.sb([128, 512], BF16, "xr") for _ in range(2)]
    xib = [P.sb([128, 512], BF16, "xi") for _ in range(2)]
    zc = P.sb([128, 4], F32, "zc")
    yT = P.sb([128, TOK], F32, "yT")
    YG0 = NHP * TOK
    ucnt = 0
    for j in range(NT):
        chq = j // 4
        P.op("dve", lambda e: e.memset(ct[:, 0:1], 1.0), writes=[ct])
        P.op("dve", lambda e: e.memset(st_[:, 0:1], 0.0), writes=[st_])
        k = 0
        w = 1
        while w < TOK:
            cK = ck[:, k * NT + j:k * NT + j + 1]
            sK = sk[:, k * NT + j:k * NT + j + 1]
            ts_(tmpA[:, 0:w], st_[:, 0:w], sK, None, ALU.mult, None, [st_, sk], [tmpA])
            P.op("dve", lambda e: e.scalar_tensor_tensor(out=ct[:, w:2 * w], in0=ct[:, 0:w], scalar=cK, in1=tmpA[:, 0:w],
                                                         op0=ALU.mult, op1=ALU.subtract), reads=[ct, ck, tmpA], writes=[ct])
            ts_(tmpA[:, 0:w], ct[:, 0:w], sK, None, ALU.mult, None, [ct, sk], [tmpA])
            P.op("dve", lambda e: e.scalar_tensor_tensor(out=st_[:, w:2 * w], in0=st_[:, 0:w], scalar=cK, in1=tmpA[:, 0:w],
                                                         op0=ALU.mult, op1=ALU.add), reads=[st_, ck, tmpA], writes=[st_])
            w *= 2
            k += 1
        c0 = ck[:, j:j + 1]
        s0 = sk[:, j:j + 1]
        Vr = Vst[:, j:j + 1]
        Vi = Vst[:, NT + j:NT + j + 1]
        ts_(zc[:, 2:3], Vi, s0, None, ALU.mult, None, [Vst, sk], [zc])
        P.op("dve", lambda e: e.scalar_tensor_tensor(out=zc[:, 0:1], in0=Vr, scalar=c0, in1=zc[:, 2:3],
                                                     op0=ALU.mult, op1=ALU.subtract), reads=[Vst, ck, zc], writes=[zc])
        ts_(zc[:, 3:4], Vr, s0, None, ALU.mult, None, [Vst, sk], [zc])
        P.op("dve", lambda e: e.scalar_tensor_tensor(out=zc[:, 1:2], in0=Vi, scalar=c0, in1=zc[:, 3:4],
                                                     op0=ALU.mult, op1=ALU.add), reads=[Vst, ck, zc], writes=[zc])
        for jc in (NCH - 1,):
            u = UT[ucnt % 2]
            ucnt += 1
            urow = (3 * NHP + chq) * 128
            P.dma("sp", u[:], PT[urow:urow + 128, jc * TOK:(jc + 1) * TOK], reads=[PTk[3 * NHP + chq]], writes=[u])
            for g in range(TOK // 512):
                gs = slice(g * 512, (g + 1) * 512)
                pr = P.next_ps()
                pi_ = P.next_ps()
                P.op("pe", lambda e: e.matmul(pr[:, :], lhsT=Bre[:, j * 128:(j + 1) * 128], rhs=u[:, gs], start=True, stop=True),
                     reads=[Bre, u], writes=[pr])
                P.op("pe", lambda e: e.matmul(pi_[:, :], lhsT=Bim[:, j * 128:(j + 1) * 128], rhs=u[:, gs], start=True, stop=True),
                     reads=[Bim, u], writes=[pi_])
                b_r = brs[g % 2]
                b_i = bis[g % 2]
                P.op("act", lambda e: e.activation(out=b_r[:], in_=pr[:, :], func=AF.Identity), reads=[pr], writes=[b_r])
                P.op("act", lambda e: e.activation(out=b_i[:], in_=pi_[:, :], func=AF.Identity), reads=[pi_], writes=[b_i])
                A_, B_ = pa[g % 2], pb_[g % 2]
                tt_(A_[:], b_r[:], ct[:, gs], ALU.mult, [b_r, ct], [A_])
                tt_(B_[:], b_i[:], st_[:, gs], ALU.mult, [b_i, st_], [B_])
                tt_(wr_[:, gs], A_[:], B_[:], ALU.add, [A_, B_], [wr_])
                C_, D_ = da[g % 2], db[g % 2]
                tt_(C_[:], b_i[:], ct[:, gs], ALU.mult, [b_i, ct], [C_])
                tt_(D_[:], b_r[:], st_[:, gs], ALU.mult, [b_r, st_], [D_])
                tt_(wi_[:, gs], C_[:], D_[:], ALU.subtract, [C_, D_], [wi_])
            rb = rho[:, j:j + 1].to_broadcast([128, TOK])
            P.op("dve", lambda e: e.tensor_tensor_scan(out=zr[:], data0=rb, data1=wr_[:], initial=zc[:, 0:1],
                                                       op0=ALU.mult, op1=ALU.add), reads=[rho, wr_, zc], writes=[zr])
            P.op("dve", lambda e: e.tensor_tensor_scan(out=zi[:], data0=rb, data1=wi_[:], initial=zc[:, 1:2],
                                                       op0=ALU.mult, op1=ALU.add), reads=[rho, wi_, zc], writes=[zi])
            if True:
                for g in range(TOK // 512):
                    gs = slice(g * 512, (g + 1) * 512)
                    A_, B_ = pa[g % 2], pb_[g % 2]
                    tt_(A_[:], zr[:, gs], ct[:, gs], ALU.mult, [zr, ct], [A_])
                    tt_(B_[:], zi[:, gs], st_[:, gs], ALU.mult, [zi, st_], [B_])
                    xr = xrb[g % 2]
                    nxi = xib[g % 2]
                    tt_(xr[:], A_[:], B_[:], ALU.subtract, [A_, B_], [xr])
                    C_, D_ = da[g % 2], db[g % 2]
                    tt_(C_[:], zr[:, gs], st_[:, gs], ALU.mult, [zr, st_], [C_])
                    tt_(D_[:], zi[:, gs], ct[:, gs], ALU.mult, [zi, ct], [D_])
                    tt_(nxi[:], C_[:], D_[:], ALU.add, [C_, D_], [nxi])
                    yp = P.next_ps()
                    P.op("pe", lambda e: e.matmul(yp[:, :], lhsT=Cr[:, j * 128:(j + 1) * 128], rhs=xr[:], start=True, stop=False),
                         reads=[Cr, xr], writes=[yp])
                    P.op("pe", lambda e: e.matmul(yp[:, :], lhsT=Ci[:, j * 128:(j + 1) * 128], rhs=nxi[:], start=False, stop=True),
                         reads=[Ci, nxi], writes=[yp])
                    if j % 4 == 0:
                        P.op("dve", lambda e: e.scalar_tensor_tensor(out=yT[:, gs], in0=u[:, gs], scalar=dsk[:, chq:chq + 1],
                                                                     in1=yp[:, :], op0=ALU.mult, op1=ALU.add),
                             reads=[u, dsk, yp], writes=[yT])
                    else:
                        tt_(yT[:, gs], yp[:, :], yT[:, gs], ALU.add, [yp, yT], [yT])
        if j % 4 == 3:
            if cfg.debug and chq < 2:
                P.dma("sp", DBG[:, (6 + chq) * TOK:(7 + chq) * TOK], yT[:], reads=[yT], writes=[DBG])
            tt_(gq[:], yT[:], yT[:], ALU.mult, [yT], [gq], eng="pool")
            ts_(gq[:], gq[:], 0.044715, 1.0, ALU.mult, ALU.add, [gq], [gq], eng="pool")
            tt_(gq[:], gq[:], yT[:], ALU.mult, [gq, yT], [gq], eng="pool")
            P.op("act", lambda e: e.activation(out=gq[:], in_=gq[:], func=AF.Sigmoid, scale=2.0 * math.sqrt(2.0 / math.pi)),
                 reads=[gq], writes=[gq])
            tt_(ymixT[:, YG0 + chq * TOK:YG0 + (chq + 1) * TOK], gq[:], yT[:], ALU.mult, [gq, yT], [ymixT], eng="pool")
    P.pop()
    wgl = P.sb([128, WC * W], BF16, "wgl")
    P.dma("pool", wgl[:], w_glu_l[:, :], reads=[w_glu_l], writes=[wgl], max_dma_last_dim=4096)
    bgl = P.sb([128, WC], F32, "bgl")
    sgc = P.sb([128, WC], F32, "sgc")
    P.dma("sp", bgl[:], b_glu_col[:, :], reads=[b_glu_col], writes=[bgl])
    P.dma("sp", sgc[:], sg_col[:, :], reads=[sg_col], writes=[sgc])
    sig = [P.sb([128, 512], F32, "sig") for _ in range(2)]
    ysg = [P.sb([128, 512], F32, "ysg") for _ in range(2)]
    ysq2 = [P.sb([128, 512], F32, "ysq2") for _ in range(2)]
    gtmp = P.sb([128, WC * 512], BF16, "gtmp")
    sss = P.sb([128, NTT], F32, "sss")
    for g in range(TOK // 512):
        for oc in range(WC):
            gp = P.next_ps()
            for k in range(WC):
                P.op("pe", lambda e: e.matmul(gp[:, :], lhsT=wgl[:, k * W + oc * 128:k * W + (oc + 1) * 128],
                                              rhs=ymixT[:, YG0 + k * TOK + g * 512:YG0 + k * TOK + (g + 1) * 512],
                                              start=(k == 0), stop=(k == WC - 1)), reads=[wgl, ymixT], writes=[gp])
            sgt = sig[oc % 2]
            yst = ysg[oc % 2]
            yq = ysq2[oc % 2]
            P.op("act", lambda e: e.activation(out=sgt[:], in_=gp[:, :], func=AF.Sigmoid, bias=bgl[:, oc:oc + 1]),
                 reads=[gp, bgl], writes=[sgt])
            tt_(yst[:], sgt[:], ymixT[:, YG0 + oc * TOK + g * 512:YG0 + oc * TOK + (g + 1) * 512], ALU.mult, [sgt, ymixT], [yst])
            if cfg.debug and oc < 2:
                P.dma("sp", DBG[:, (4 + oc) * TOK + g * 512:(4 + oc) * TOK + (g + 1) * 512], yst[:], reads=[yst], writes=[DBG])
            tt_(yq[:], yst[:], yst[:], ALU.mult, [yst], [yq], eng="pool")
            sp_ = P.next_ps()
            for t4 in range(4):
                P.op("pe", lambda e: e.matmul(sp_[:, t4:t4 + 1], lhsT=yq[:, t4 * 128:(t4 + 1) * 128], rhs=ones_f[:, 0:1],
                                              start=True, stop=True), reads=[yq, ones_f], writes=[sp_])
            if oc == 0:
                P.op("dve", lambda e: e.tensor_copy(out=sss[:, g * 4:(g + 1) * 4], in_=sp_[:, 0:4]), reads=[sp_], writes=[sss])
            else:
                tt_(sss[:, g * 4:(g + 1) * 4], sp_[:, 0:4], sss[:, g * 4:(g + 1) * 4], ALU.add, [sp_, sss], [sss])
            P.op("act", lambda e: e.activation(out=gtmp[:, oc * 512:(oc + 1) * 512], in_=yst[:], func=AF.Identity,
                                               scale=sgc[:, oc:oc + 1]), reads=[yst, sgc], writes=[gtmp])
        for oc in range(WC):
            P.op("pool", lambda e: e.tensor_copy(out=ymixT[:, YG0 + oc * TOK + g * 512:YG0 + oc * TOK + (g + 1) * 512],
                                                 in_=gtmp[:, oc * 512:(oc + 1) * 512]), reads=[gtmp], writes=[ymixT])
    rstd_from_ss(sss, sss[:], W, rstd_s, rstd_s[:], width=NTT)
    P.pop()

    P.push()
    NK = NHP + WC
    wo = P.sb([128, NK * D], BF16, "wo")
    for k in range(NK):
        P.dma("pool", wo[:, k * D:(k + 1) * D], w_out_l[:, k * D:(k + 1) * D], reads=[w_out_l], writes=[wo],
              max_dma_last_dim=4096)
    g1b = P.sb([128, D], F32, "g1b")
    P.dma("sp", g1b[:], modrow[2 * DC:3 * DC, :].rearrange("(o a) b -> o (a b)", o=1).partition_broadcast(128), reads=[modrow], writes=[g1b])
    wr_f = P.sb([128, DC * E], F32, "wrf")
    P.dma("sp", wr_f[:], w_r_l[:, :], reads=[w_r_l], writes=[wr_f])
    brb = P.sb([128, E], F32, "brb")
    P.dma("sp", brb[:], b_r_row[0:1, :].partition_broadcast(128), reads=[b_r_row], writes=[brb])
    xo = [P.sb([128, D], F32, "xo") for _ in range(2)]
    x2 = [P.sb([128, D], F32, "x2") for _ in range(2)]
    ta = [P.sb([128, 512], F32, "ta") for _ in range(2)]
    xn2 = P.sb([128, D], F32, "xn2")
    h2f = P.sb([128, DC * 128], F32, "h2f")
    h2b = [P.sb([128, DC * 128], BF16, "h2b") for _ in range(2)]
    ss2 = P.sb([128, 1], F32, "ss2")
    rs2 = P.sb([128, 1], F32, "rs2")
    lg = P.sb([128, E], F32, "lg")
    m8 = P.sb([128, 8], F32, "m8")
    nmx = P.sb([128, 1], F32, "nmx")
    msk = P.sb([128, E], F32, "msk")
    ex = P.sb([128, E], F32, "ex")
    dn = P.sb([128, 1], F32, "dn")
    OWN = (NCH - 1) * TOK
    for tt in range(NTT):
        i = tt % 2
        P.dma("sp", xo[i][:], x_ext[OWN + tt * 128:OWN + (tt + 1) * 128, :], reads=[x_ext], writes=[xo[i]])
        for cg in range(D // 512):
            cs = slice(cg * 512, (cg + 1) * 512)
            pA = P.next_ps()
            pS = P.next_ps()
            for k in range(NHP):
                P.op("pe", lambda e: e.matmul(pA[:, :], lhsT=ymixT[:, k * TOK + tt * 128:k * TOK + (tt + 1) * 128],
                                              rhs=wo[:, k * D + cg * 512:k * D + (cg + 1) * 512],
                                              start=(k == 0), stop=(k == NHP - 1)), reads=[ymixT, wo], writes=[pA])
            for k in range(NHP, NK):
                P.op("pe", lambda e: e.matmul(pS[:, :], lhsT=ymixT[:, k * TOK + tt * 128:k * TOK + (tt + 1) * 128],
                                              rhs=wo[:, k * D + cg * 512:k * D + (cg + 1) * 512],
                                              start=(k == NHP), stop=(k == NK - 1)), reads=[ymixT, wo], writes=[pS])
            t_ = ta[cg % 2]
            ts_(t_[:], pA[:, :], rstd_a[:, tt:tt + 1], None, ALU.mult, None, [pA, rstd_a], [t_])
            P.op("dve", lambda e: e.scalar_tensor_tensor(out=t_[:], in0=pS[:, :], scalar=rstd_s[:, tt:tt + 1], in1=t_[:],
                                                         op0=ALU.mult, op1=ALU.add), reads=[pS, rstd_s, t_], writes=[t_])
            tt_(t_[:], t_[:], g1b[:, cs], ALU.mult, [t_, g1b], [t_])
            tt_(x2[i][:, cs], t_[:], xo[i][:, cs], ALU.add, [t_, xo[i]], [x2[i]])
        P.dma("sp", X2[tt * 128:(tt + 1) * 128, :], x2[i][:], reads=[x2[i]], writes=[X2k[tt]])
        P.op("act", lambda e: e.activation(out=xn2[:], in_=x2[i][:], func=AF.Square, accum_out=ss2[:, 0:1]),
             reads=[x2[i]], writes=[xn2, ss2])
        rstd_from_ss(ss2, ss2[:, 0:1], D, rs2, rs2[:, 0:1])
        P.op("dve", lambda e: e.tensor_scalar(out=xn2[:], in0=x2[i][:], scalar1=rs2[:, 0:1], scalar2=None, op0=ALU.mult),
             reads=[x2[i], rs2], writes=[xn2])
        hb = h2b[i]
        for kb in range(0, DC, 4):
            tp = P.next_ps()
            nk = min(4, DC - kb)
            for kk in range(nk):
                k = kb + kk
                P.op("pe", lambda e: e.transpose(tp[:, kk * 128:(kk + 1) * 128], xn2[:, k * 128:(k + 1) * 128], ident_f[:]),
                     reads=[xn2, ident_f], writes=[tp])
            for kk in range(nk):
                k = kb + kk
                P.op("dve", lambda e: e.tensor_scalar(out=h2f[:, k * 128:(k + 1) * 128], in0=tp[:, kk * 128:(kk + 1) * 128],
                                                      scalar1=s2c[:, k:k + 1], scalar2=modT[:, 3 * DC + k:3 * DC + k + 1],
                                                      op0=ALU.mult, op1=ALU.add), reads=[tp, s2c, modT], writes=[h2f])
        P.op("act", lambda e: e.activation(out=hb[:], in_=h2f[:], func=AF.Identity), reads=[h2f], writes=[hb])
        P.dma("sp", H2T[:, tt * 128:(tt + 1) * 128].rearrange("(k p) t -> p k t", p=128),
              hb[:, :].rearrange("p (k t) -> p k t", k=DC), reads=[hb], writes=[H2T])
        lp = P.next_ps()
        for k in range(DC):
            P.op("pe", lambda e: e.matmul(lp[:, 0:E], lhsT=h2f[:, k * 128:(k + 1) * 128], rhs=wr_f[:, k * E:(k + 1) * E],
                                          start=(k == 0), stop=(k == DC - 1)), reads=[h2f, wr_f], writes=[lp])
        tt_(lg[:], lp[:, 0:E], brb[:], ALU.add, [lp, brb], [lg])
        P.op("dve", lambda e: e.max(out=m8[:], in_=lg[:]), reads=[lg], writes=[m8])
        ts_(msk[:], lg[:], m8[:, TOP_K - 1:TOP_K], None, ALU.is_ge, None, [lg, m8], [msk])
        ts_(nmx[:], m8[:, 0:1], -1.0, None, ALU.mult, None, [m8], [nmx])
        P.op("act", lambda e: e.activation(out=ex[:], in_=lg[:], func=AF.Exp, bias=nmx[:, 0:1]), reads=[lg, nmx], writes=[ex])
        tt_(ex[:], ex[:], msk[:], ALU.mult, [ex, msk], [ex])
        P.op("dve", lambda e: e.reduce_sum(out=dn[:], in_=ex[:], axis=AX.X), reads=[ex], writes=[dn])
        P.op("dve", lambda e: e.reciprocal(out=dn[:], in_=dn[:]), reads=[dn], writes=[dn])
        ts_(G[:, tt * E:(tt + 1) * E], ex[:], dn[:, 0:1], None, ALU.mult, None, [ex, dn], [G])
    if cfg.debug:
        GD = P.dram("GD", [128, NTT * E], F32, kind=SK)
        P.dma("sp", GD[:, :], G[:], reads=[G], writes=[GD])
    P.pop()
    P.pop()

    HALF = TOK // 2
    HT = HALF // 128
    NG = HALF // 512
    NCG = D // 512
    for hf in range(2):
        P.push()
        yacc = P.sb([128, HT * D], F32, "yacc")
        P.push()
        GT = P.sb([E, HALF], F32, "GT")
        bdn = P.sb([E, D], F32, "bdn")
        P.dma("sp", bdn[:], b_down[:, :], reads=[b_down], writes=[bdn])
        for t8 in range(HT):
            tg = hf * HT + t8
            tp = P.next_ps()
            P.op("pe", lambda e: e.transpose(tp[0:E, 0:128], G[:, tg * E:(tg + 1) * E], ident_f[:]),
                 reads=[G, ident_f], writes=[tp])
            P.op("dve", lambda e: e.tensor_copy(out=GT[:, t8 * 128:(t8 + 1) * 128], in_=tp[0:E, 0:128]),
                 reads=[tp], writes=[GT])
        for t8 in range(HT):
            for cg in range(NCG):
                bp = P.next_ps()
                P.op("pe", lambda e: e.matmul(bp[:, :], lhsT=GT[:, t8 * 128:(t8 + 1) * 128], rhs=bdn[:, cg * 512:(cg + 1) * 512],
                                              start=True, stop=True), reads=[GT, bdn], writes=[bp])
                P.op("act", lambda e: e.activation(out=yacc[:, t8 * D + cg * 512:t8 * D + (cg + 1) * 512], in_=bp[:, :],
                                                   func=AF.Identity), reads=[bp], writes=[yacc])
        P.pop()
        P.push()
        bgc = P.sb([128, E * FC], F32, "bgc")
        blc = P.sb([128, E * FC], F32, "blc")
        P.dma("sp", bgc[:], bg_col[:, :], reads=[bg_col], writes=[bgc])
        P.dma("sp", blc[:], bl_col[:, :], reads=[bl_col], writes=[blc])
        h2h = P.sb([128, DC * HALF], BF16, "h2h")
        actT = P.sb([128, FC * HALF], BF16, "actT")
        wg = [P.sb([128, DC * 128], BF16, "wg") for _ in range(2)]
        wl = [P.sb([128, DC * 128], BF16, "wl") for _ in range(2)]
        wd = [P.sb([128, FC * 512], BF16, "wd") for _ in range(2)]
        gclb = [P.sb([128, 512], F32, "gcl") for _ in range(2)]
        sgmb = [P.sb([128, 512], F32, "sgm") for _ in range(2)]
        lclb = [P.sb([128, 512], F32, "lcl") for _ in range(2)]
        ecnt = 0
        P.dma("sp", h2h[:, :].rearrange("p (k t) -> p k t", k=DC),
              H2T[:, hf * HALF:(hf + 1) * HALF].rearrange("(k p) t -> p k t", p=128), reads=[H2T], writes=[h2h])
        wc = 0
        dcn = 0
        for ex_ in range(E):
            for fc in range(FC):
                wgt = wg[wc % 2]
                wlt = wl[wc % 2]
                wc += 1
                P.dma("pool", wgt[:], w_gate_l[ex_ * FC + fc, :, :], reads=[w_gate_l], writes=[wgt])
                P.dma("pool", wlt[:], w_lin_l[ex_ * FC + fc, :, :], reads=[w_lin_l], writes=[wlt])
                col = ex_ * FC + fc
                for g in range(NG):
                    gp = P.next_ps()
                    lp = P.next_ps()
                    for k in range(DC):
                        P.op("pe", lambda e: e.matmul(gp[:, :], lhsT=wgt[:, k * 128:(k + 1) * 128],
                                                      rhs=h2h[:, k * HALF + g * 512:k * HALF + (g + 1) * 512],
                                                      start=(k == 0), stop=(k == DC - 1)), reads=[wgt, h2h], writes=[gp])
                    for k in range(DC):
                        P.op("pe", lambda e: e.matmul(lp[:, :], lhsT=wlt[:, k * 128:(k + 1) * 128],
                                                      rhs=h2h[:, k * HALF + g * 512:k * HALF + (g + 1) * 512],
                                                      start=(k == 0), stop=(k == DC - 1)), reads=[wlt, h2h], writes=[lp])
                    gcl, sgm, lcl = gclb[ecnt % 2], sgmb[ecnt % 2], lclb[ecnt % 2]
                    ecnt += 1
                    ts_(gcl[:], gp[:, :], bgc[:, col:col + 1], SW_LIMIT, ALU.add, ALU.min, [gp, bgc], [gcl])
                    P.op("act", lambda e: e.activation(out=sgm[:], in_=gcl[:], func=AF.Sigmoid, scale=SW_ALPHA),
                         reads=[gcl], writes=[sgm])
                    ts_(lcl[:], lp[:, :], blc[:, col:col + 1], SW_LIMIT, ALU.add, ALU.min, [lp, blc], [lcl])
                    ts_(lcl[:], lcl[:], -SW_LIMIT, 1.0, ALU.max, ALU.add, [lcl], [lcl])
                    tt_(sgm[:], gcl[:], sgm[:], ALU.mult, [gcl, sgm], [sgm])
                    tt_(actT[:, fc * HALF + g * 512:fc * HALF + (g + 1) * 512], sgm[:], lcl[:], ALU.mult,
                        [sgm, lcl], [actT])
            for cg in range(NCG):
                wdt = wd[dcn % 2]
                dcn += 1
                P.dma("pool", wdt[:, :].rearrange("p (f c) -> p f c", f=FC),
                      w_down_l[ex_ * FC:(ex_ + 1) * FC, :, cg * 512:(cg + 1) * 512].rearrange("f p c -> p f c"),
                      reads=[w_down_l], writes=[wdt])
                for t8 in range(HT):
                    tg = hf * HT + t8
                    yp = P.next_ps()
                    for fc in range(FC):
                        P.op("pe", lambda e: e.matmul(yp[:, :], lhsT=actT[:, fc * HALF + t8 * 128:fc * HALF + (t8 + 1) * 128],
                                                      rhs=wdt[:, fc * 512:(fc + 1) * 512],
                                                      start=(fc == 0), stop=(fc == FC - 1)), reads=[actT, wdt], writes=[yp])
                    ysl = yacc[:, t8 * D + cg * 512:t8 * D + (cg + 1) * 512]
                    P.op("dve", lambda e: e.scalar_tensor_tensor(out=ysl, in0=yp[:, :], scalar=G[:, tg * E + ex_:tg * E + ex_ + 1],
                                                                 in1=ysl, op0=ALU.mult, op1=ALU.add),
                         reads=[yp, G, yacc], writes=[yacc])
        P.pop()
        P.push()
        g2b = P.sb([128, D], F32, "g2b")
        P.dma("sp", g2b[:], modrow[5 * DC:6 * DC, :].rearrange("(o a) b -> o (a b)", o=1).partition_broadcast(128),
              reads=[modrow], writes=[g2b])
        fgb = P.sb([128, D], F32, "fgb")
        P.dma("sp", fgb[:], fg_row[0:1, :].partition_broadcast(128), reads=[fg_row], writes=[fgb])
        x2t = [P.sb([128, D], F32, "x2t") for _ in range(2)]
        junk3 = P.sb([128, D], BF16, "junk3")
        ss3 = P.sb([128, 1], F32, "ss3")
        rs3 = P.sb([128, 1], F32, "rs3")
        for t8 in range(HT):
            tg = hf * HT + t8
            xx = x2t[t8 % 2]
            P.dma("sp", xx[:], X2[tg * 128:(tg + 1) * 128, :], reads=[X2k[tg]], writes=[xx])
            ysl = yacc[:, t8 * D:(t8 + 1) * D]
            tt_(ysl, ysl, g2b[:], ALU.mult, [yacc, g2b], [yacc])
            tt_(xx[:], xx[:], ysl, ALU.add, [xx, yacc], [xx])
            P.op("act", lambda e: e.activation(out=junk3[:], in_=xx[:], func=AF.Square, accum_out=ss3[:, 0:1]),
                 reads=[xx], writes=[junk3, ss3])
            rstd_from_ss(ss3, ss3[:, 0:1], D, rs3, rs3[:, 0:1])
            P.op("dve", lambda e: e.scalar_tensor_tensor(out=xx[:], in0=xx[:], scalar=rs3[:, 0:1], in1=fgb[:],
                                                         op0=ALU.mult, op1=ALU.mult), reads=[xx, rs3, fgb], writes=[xx])
            P.dma("sp", out_d[tg * 128:(tg + 1) * 128, :], xx[:], reads=[xx], writes=[outk[tg]])
        P.pop()
        P.pop()
    P.close()
    return nc


def _col(v, nchunks):
    return np.ascontiguousarray(np.asarray(v, np.float32).reshape(nchunks, 128).T)


def _wchunks(w, ncol_chunks):
    K, N = w.shape
    a = np.asarray(w, np.float32).reshape(K // 128, 128, ncol_chunks, 128)
    return np.ascontiguousarray(a.transpose(2, 1, 0, 3).reshape(ncol_chunks, 128, (K // 128) * 128))


def _rows(w):
    K, N = w.shape
    a = np.asarray(w, np.float32).reshape(K // 128, 128, N)
    return np.ascontiguousarray(a.transpose(1, 0, 2).reshape(128, (K // 128) * N))


def prepare_inputs(cfg, x, c, w_ada, b_ada, norm1_g, w_in, lambda_re, lambda_im, ssm_b_re, ssm_b_im,
                   ssm_c_re, ssm_c_im, ssm_d, ssm_log_dt, w_glu, b_glu, attn_out_g, ssm_out_g,
                   w_out, norm2_g, w_router, b_router, w_gate_up, b_gate_up, w_down, b_down, final_g):
    D, TOK, NCH, NHP, WC, NT, DC, E, F, FC, INC = (cfg.D, cfg.TOK, cfg.NCH, cfg.NHP, cfg.WC, cfg.NT, cfg.DC,
                                                    cfg.E, cfg.F, cfg.FC, cfg.INC)
    f32 = np.float32
    x = np.asarray(x, f32)[0]
    L = 0
    G_ = cfg.W // 16
    common = {}
    kq = np.arange(128)
    mcur = (kq[:, None] <= kq[None, :]).astype(f32)
    mprev = (kq[:, None] >= kq[None, :]).astype(f32)
    common["mask_full"] = np.concatenate([mcur, mprev], axis=1)
    common["ident"] = np.eye(128, dtype=f32)
    common["c_col"] = _col(np.asarray(c, f32)[0], DC)
    common["w_ada_l"] = _wchunks(np.asarray(w_ada, f32)[L], 6 * DC)
    common["b_ada_col"] = _col(np.asarray(b_ada, f32)[L], 6 * DC)
    common["n1g_col"] = _col(np.asarray(norm1_g, f32)[L], DC)
    common["n1g_row"] = np.asarray(norm1_g, f32)[L].reshape(1, D)
    common["n2g_col"] = _col(np.asarray(norm2_g, f32)[L], DC)
    common["fg_row"] = np.asarray(final_g, f32).reshape(1, D)
    common["w_in_l"] = _wchunks(np.asarray(w_in, f32)[L], INC)

    def st_layout(a):
        a = np.asarray(a, f32).reshape(NT, 2, 64)
        return np.ascontiguousarray(a.transpose(1, 2, 0).reshape(128, NT))
    common["lam_re_l"] = st_layout(np.asarray(lambda_re, f32)[L])
    common["lam_im_l"] = st_layout(np.asarray(lambda_im, f32)[L])
    common["logdt_l"] = st_layout(np.repeat(np.asarray(ssm_log_dt, f32)[L][:, None], 64, axis=1))
    bre = np.zeros((128, NT, 128), f32)
    bim = np.zeros((128, NT, 128), f32)
    cre = np.zeros((128, NT, 128), f32)
    cim = np.zeros((128, NT, 128), f32)
    b_re = np.asarray(ssm_b_re, f32)[L]
    b_im = np.asarray(ssm_b_im, f32)[L]
    c_re = np.asarray(ssm_c_re, f32)[L]
    c_im = np.asarray(ssm_c_im, f32)[L]
    for j in range(NT):
        for gp in range(2):
            g = 2 * j + gp
            ch0 = 16 * (2 * (j % 4) + gp)
            bre[ch0:ch0 + 16, j, gp * 64:(gp + 1) * 64] = b_re[g].T
            bim[ch0:ch0 + 16, j, gp * 64:(gp + 1) * 64] = b_im[g].T
            cre[gp * 64:(gp + 1) * 64, j, ch0:ch0 + 16] = c_re[g].T
            cim[gp * 64:(gp + 1) * 64, j, ch0:ch0 + 16] = c_im[g].T
    common["bre_l"] = bre.reshape(128, NT * 128)
    common["bim_l"] = bim.reshape(128, NT * 128)
    common["cre_l"] = cre.reshape(128, NT * 128)
    common["cim_l"] = cim.reshape(128, NT * 128)
    common["dskip_col"] = _col(np.asarray(ssm_d, f32)[L].reshape(-1), WC)
    common["w_glu_l"] = _rows(np.asarray(w_glu, f32)[L])
    common["b_glu_col"] = _col(np.asarray(b_glu, f32)[L], WC)
    common["ag_col"] = _col(np.asarray(attn_out_g, f32)[L], NHP)
    common["sg_col"] = _col(np.asarray(ssm_out_g, f32)[L], WC)
    common["w_out_l"] = _rows(np.asarray(w_out, f32)[L])
    common["w_r_l"] = _rows(np.asarray(w_router, f32)[L])
    common["b_r_row"] = np.asarray(b_router, f32)[L].reshape(1, E)
    wgu = np.asarray(w_gate_up, f32)[L]
    wgl_ = np.empty((E * FC, 128, DC * 128), f32)
    wll_ = np.empty((E * FC, 128, DC * 128), f32)
    for e in range(E):
        wgl_[e * FC:(e + 1) * FC] = _wchunks(wgu[e][:, 0::2], FC)
        wll_[e * FC:(e + 1) * FC] = _wchunks(wgu[e][:, 1::2], FC)
    common["w_gate_l"] = wgl_
    common["w_lin_l"] = wll_
    bgu = np.asarray(b_gate_up, f32)[L]
    common["bg_col"] = _col(bgu[:, 0::2].reshape(-1), E * FC)
    common["bl_col"] = _col(bgu[:, 1::2].reshape(-1), E * FC)
    common["w_down_l"] = np.ascontiguousarray(np.asarray(w_down, f32)[L].reshape(E * FC, 128, D))
    common["b_down"] = np.ascontiguousarray(np.asarray(b_down, f32)[L])
    in_maps = []
    for i in range(NCORES):
        m = dict(common)
        xe = np.zeros((NCH * TOK, D), f32)
        val = np.zeros((128, NCH), f32)
        for jc in range(NCH):
            gchunk = i - (NCH - 1) + jc
            if gchunk >= 0:
                xe[jc * TOK:(jc + 1) * TOK] = x[gchunk * TOK:(gchunk + 1) * TOK]
                val[:, jc] = 1.0
        m["x_ext"] = xe
        m["valid"] = val
        mf = common["mask_full"].copy()
        if i == 0:
            mf[:, 128:256] = 0.0
        m["mask_first"] = mf
        in_maps.append(m)
    return in_maps


_NC_CACHE = {}


def run(cfg, inputs):
    key = (cfg.D, cfg.TOK, cfg.E, cfg.F)
    if key not in _NC_CACHE:
        _NC_CACHE[key] = build_program(cfg)
    nc = _NC_CACHE[key]
    in_maps = prepare_inputs(cfg, **inputs)
    res = run_bass_kernel_spmd(nc, in_maps, core_ids=list(range(NCORES)))
    global LAST_RES
    LAST_RES = res
    out = np.concatenate([np.asarray(r["out"], np.float32) for r in res.results], axis=0)
    return out.reshape(1, NCORES * cfg.TOK, cfg.D)


def kernel(**inputs):
    cfg = Cfg(D=2048, TOK=2048, E=32, F=2048)
    return run(cfg, inputs)
```
